# Optimizing a Trainium2 kernel written in Bass

```python
import math
import jax, jax.numpy as jnp
from jax import lax
import numpy as np

D_MODEL = 1024
BATCH = 2
SEQ = 16384
DEPTH = 2

GRID_W = 64
CTX_LEN = 256
Q_BLOCK = 128
ROPE_BASE = 10000.0
EPS = 1e-6

DIFF_HEADS = 4
DIFF_HEAD_DIM = 64
DIFF_V_DIM = 2 * DIFF_HEAD_DIM
HYENA_WIDTH = 256
HYENA_ORDER = 2
HYENA_SHORT = 3
FILT_EMB = 33
FILT_HIDDEN = 64
DECAY_TARGET = 1e-2
FAST_DECAY = 0.3
SLOW_DECAY = 1.5
CONF_WIDTH = 256
CONF_KERNEL = 31
MLA_HEADS = 4
MLA_Q_RANK = 256
MLA_KV_RANK = 128
MLA_NOPE = 64
MLA_ROPE = 32
MLA_V = 64
MLA_SCALE = (MLA_NOPE + MLA_ROPE) ** -0.5
N_BRANCH = 4
N_EXPERTS = 16
EC_CAPACITY = 2
EXPERT_HIDDEN = 1024

DIFF_QK_W = DIFF_HEADS * 2 * DIFF_HEAD_DIM
DIFF_V_W = DIFF_HEADS * DIFF_V_DIM
HYENA_PROJ = (HYENA_ORDER + 1) * HYENA_WIDTH
CONF_PROJ = 2 * CONF_WIDTH
GATE_PROJ = N_BRANCH * D_MODEL
IN_SPLITS = (DIFF_QK_W, DIFF_QK_W, DIFF_V_W, HYENA_PROJ, CONF_PROJ, MLA_Q_RANK, MLA_KV_RANK, MLA_ROPE, GATE_PROJ)
IN_WIDTH = DIFF_QK_W * 2 + DIFF_V_W + HYENA_PROJ + CONF_PROJ + MLA_Q_RANK + MLA_KV_RANK + MLA_ROPE + GATE_PROJ
BRANCH_WIDTHS = (DIFF_V_W, HYENA_WIDTH, CONF_WIDTH, MLA_HEADS * MLA_V)
MIX_WIDTH = DIFF_V_W + HYENA_WIDTH + CONF_WIDTH + MLA_HEADS * MLA_V

kernel_name = 'hybrid_diffusion_parallel_mixers_ec_moe'


def _split(z, sizes):
    out, start = [], 0
    for s in sizes:
        out.append(z[..., start:start + s])
        start += s
    return out


def rms_norm(x, g):
    xf = x.astype(jnp.float32)
    y = xf * lax.rsqrt(jnp.mean(xf * xf, axis=-1, keepdims=True) + EPS)
    return y.astype(x.dtype) * g


def layer_norm(x, g, b):
    xf = x.astype(jnp.float32)
    mu = jnp.mean(xf, axis=-1, keepdims=True)
    var = jnp.mean(jnp.square(xf - mu), axis=-1, keepdims=True)
    return ((xf - mu) * lax.rsqrt(var + EPS)).astype(x.dtype) * g + b


def modulate(x, shift, scale):
    return x * (1.0 + scale) + shift


def axial_rope_tables(n_tok, rot_dim):
    rows = n_tok // GRID_W
    row = jnp.repeat(jnp.arange(rows), GRID_W).astype(jnp.float32)
    col = jnp.tile(jnp.arange(GRID_W), rows).astype(jnp.float32)
    nf = rot_dim // 4
    inv = ROPE_BASE ** (-jnp.arange(nf, dtype=jnp.float32) / nf)
    ang = jnp.concatenate([row[:, None] * inv, col[:, None] * inv], axis=-1)
    return jnp.cos(ang), jnp.sin(ang)


def apply_rope(x, cos, sin):
    half = x.shape[-1] // 2
    x1, x2 = x[..., :half], x[..., half:]
    return jnp.concatenate([x1 * cos - x2 * sin, x1 * sin + x2 * cos], axis=-1).astype(x.dtype)


def depthwise_conv(u, w):
    k, c = w.shape
    return lax.conv_general_dilated(u, w[:, None, :].astype(u.dtype), window_strides=(1,),
                                    padding=[(k // 2, k // 2)], dimension_numbers=('NWC', 'WIO', 'NWC'),
                                    feature_group_count=c)


def attn_probs(q, k, scale):
    s = jnp.einsum('...qd,...kd->...qk', q, k).astype(jnp.float32) * scale
    return jax.nn.softmax(s, axis=-1)


def softmax_attend(q, k, v, scale):
    p = attn_probs(q, k, scale)
    return jnp.einsum('bhqk,bhkv->bhqv', p.astype(v.dtype), v)


def diff_attend(q, k, v, lam):
    p = attn_probs(q, k, DIFF_HEAD_DIM ** -0.5)
    a = p[:, :, 0] - lam * p[:, :, 1]
    return jnp.einsum('bhqk,bhkv->bhqv', a.astype(v.dtype), v)


def sweep_query_blocks(fn, q):
    *lead, s, d = q.shape
    nb = s // Q_BLOCK
    qb = jnp.moveaxis(q.reshape(*lead, nb, Q_BLOCK, d), -3, 0)
    out = jnp.moveaxis(lax.map(fn, qb), 0, -3)
    return out.reshape(*out.shape[:-3], s, out.shape[-1])


def diff_heads(z):
    b, n, _ = z.shape
    return z.reshape(b, n, DIFF_HEADS, 2, DIFF_HEAD_DIM).transpose(0, 2, 3, 1, 4)


def value_heads(z, n_heads):
    b, n, _ = z.shape
    return z.reshape(b, n, n_heads, -1).transpose(0, 2, 1, 3)


def merge_heads(o):
    b, h, n, dv = o.shape
    return o.transpose(0, 2, 1, 3).reshape(b, n, h * dv)


def diff_finish(o, g, lam_init):
    return merge_heads(rms_norm(o, g) * (1.0 - lam_init))


def mla_queries(cq, p, rope):
    q = value_heads(rms_norm(cq, p['mla_q_norm_g']) @ p['mla_w_uq'], MLA_HEADS)
    q_nope, q_rope = q[..., :MLA_NOPE], q[..., MLA_NOPE:]
    if rope is not None:
        q_rope = apply_rope(q_rope, *rope)
    return jnp.concatenate([q_nope, q_rope], axis=-1)


def mla_keys_values(ckv, kr, p, rope):
    kv = value_heads(rms_norm(ckv, p['mla_kv_norm_g']) @ p['mla_w_ukv'], MLA_HEADS)
    k_nope, v = kv[..., :MLA_NOPE], kv[..., MLA_NOPE:]
    kr = kr[:, None]
    if rope is not None:
        kr = apply_rope(kr, *rope)
    k = jnp.concatenate([k_nope, jnp.broadcast_to(kr, k_nope.shape[:-1] + (MLA_ROPE,))], axis=-1)
    return k, v


def hyena_filters(n, p):
    t = jnp.linspace(0.0, 1.0, n, dtype=jnp.float32)[:, None]
    bands = (FILT_EMB - 1) // 2
    w = (2.0 * math.pi / n) * jnp.arange(n, dtype=jnp.float32)[:, None]
    f = jnp.linspace(1e-4, bands - 1, bands, dtype=jnp.float32)[None, :]
    z = jnp.concatenate([t, jnp.cos(f * w), -jnp.sin(f * w)], axis=-1)
    hid = jnp.sin(p['filt_freq'][0] * (z @ p['filt_w1'] + p['filt_b1']))
    hid = jnp.sin(p['filt_freq'][1] * (hid @ p['filt_w2'] + p['filt_b2']))
    h = (hid @ p['filt_w3']).astype(jnp.float32).reshape(n, HYENA_ORDER, 2, HYENA_WIDTH)
    deltas = jnp.abs(jnp.linspace(math.log(DECAY_TARGET) / SLOW_DECAY, math.log(DECAY_TARGET) / FAST_DECAY,
                                  HYENA_WIDTH, dtype=jnp.float32))
    h = h * jnp.exp(-t * deltas)[:, None, None, :]
    return h / jnp.sum(jnp.abs(h), axis=0, keepdims=True)


def centred_long_conv(u, h_fwd, h_bwd):
    n = u.shape[1]
    k = jnp.concatenate([h_fwd, jnp.zeros_like(h_fwd[:1]), h_bwd[:-1][::-1]], axis=0)
    u_f = jnp.fft.rfft(u.astype(jnp.float32), n=2 * n, axis=1)
    k_f = jnp.fft.rfft(k, axis=0)
    y = jnp.fft.irfft(u_f * k_f[None], n=2 * n, axis=1)[:, :n]
    return y.astype(u.dtype)


def hyena_branch(z, p):
    n = z.shape[1]
    z = depthwise_conv(z, p['hyena_short_w']) + p['hyena_short_b']
    x1, x2, v = _split(z, (HYENA_WIDTH, HYENA_WIDTH, HYENA_WIDTH))
    h = hyena_filters(n, p)
    for o, gate in enumerate((x1, x2)):
        v = gate * (centred_long_conv(v, h[:, o, 0], h[:, o, 1]) + p['hyena_skip'][o] * v)
    return v


def conformer_branch(z, p):
    a, b = _split(z, (CONF_WIDTH, CONF_WIDTH))
    u = depthwise_conv(a * jax.nn.sigmoid(b), p['conf_dw_w'])
    return jax.nn.silu(layer_norm(u, p['conf_ln_g'], p['conf_ln_b']))


def merge_branches(ys, g, p):
    gates = jax.nn.sigmoid(g.reshape(*g.shape[:-1], N_BRANCH, D_MODEL))
    acc, start = 0.0, 0
    for i, (y, w) in enumerate(zip(ys, BRANCH_WIDTHS)):
        acc = acc + gates[..., i, :] * (y @ p['w_branch'][start:start + w])
        start += w
    return acc @ p['w_out']


def token_mixer(u_lat, u_ctx, p, lam, lam_init, rope_diff, rope_mla, with_ctx_out):
    dq_l, dk_l, dv_l, hy_l, cf_l, cq_l, ckv_l, kr_l, g_l = _split(u_lat @ p['w_in'], IN_SPLITS)
    dq_c, dk_c, dv_c, hy_c, cf_c, cq_c, ckv_c, kr_c, g_c = _split(u_ctx @ p['w_in'], IN_SPLITS)
    dk_c, dv_c = diff_heads(dk_c), value_heads(dv_c, DIFF_HEADS)
    mk_c, mv_c = mla_keys_values(ckv_c, kr_c, p, None)
    dk_all = jnp.concatenate([dk_c, apply_rope(diff_heads(dk_l), *rope_diff)], axis=-2)
    dv_all = jnp.concatenate([dv_c, value_heads(dv_l, DIFF_HEADS)], axis=-2)
    mk_l, mv_l = mla_keys_values(ckv_l, kr_l, p, rope_mla)
    mk_all = jnp.concatenate([mk_c, mk_l], axis=-2)
    mv_all = jnp.concatenate([mv_c, mv_l], axis=-2)
    dq = apply_rope(diff_heads(dq_l), *rope_diff)
    a_lat = sweep_query_blocks(lambda qb: diff_attend(qb, dk_all, dv_all, lam), dq)
    m_lat = sweep_query_blocks(lambda qb: softmax_attend(qb, mk_all, mv_all, MLA_SCALE), mla_queries(cq_l, p, rope_mla))
    y_lat = merge_branches((diff_finish(a_lat, p['diff_subln_g'], lam_init), hyena_branch(hy_l, p),
                            conformer_branch(cf_l, p), merge_heads(m_lat)), g_l, p)
    if not with_ctx_out:
        return y_lat, None
    a_ctx = diff_attend(diff_heads(dq_c), dk_c, dv_c, lam)
    m_ctx = softmax_attend(mla_queries(cq_c, p, None), mk_c, mv_c, MLA_SCALE)
    y_ctx = merge_branches((diff_finish(a_ctx, p['diff_subln_g'], lam_init), hyena_branch(hy_c, p),
                            conformer_branch(cf_c, p), merge_heads(m_ctx)), g_c, p)
    return y_lat, y_ctx


def expert_choice_ffn(u, p):
    b, n, d = u.shape
    cap = max(1, EC_CAPACITY * n // N_EXPERTS)
    aff = jax.nn.softmax((u @ p['w_router']).astype(jnp.float32), axis=-1)
    gate, idx = lax.top_k(jnp.swapaxes(aff, 1, 2), cap)
    xe = jax.vmap(lambda ub, ib: ub[ib])(u, idx)
    hg, hu = jnp.split(jnp.einsum('becd,edf->becf', xe, p['w_exp_in']), 2, axis=-1)
    ye = jnp.einsum('becf,efd->becd', jax.nn.silu(hg) * hu, p['w_exp_out']) * gate[..., None].astype(u.dtype)
    return jax.vmap(lambda yb, ib: jnp.zeros((n, d), yb.dtype).at[ib.reshape(-1)].add(yb.reshape(-1, d)))(ye, idx)


def setup_inputs(seed: int = 0) -> dict:
    key = jax.random.key(seed)
    ks = iter(jax.random.split(key, 40))

    def nrm(shape, scale):
        return scale * jax.random.normal(next(ks), shape, jnp.float32)

    def gain(shape):
        return 1.0 + nrm(shape, 0.02)

    L, D = DEPTH, D_MODEL
    return {
        'x': nrm((BATCH, SEQ, D), 1.0),
        'c': nrm((BATCH, D), 1.0),
        'ctx': nrm((BATCH, CTX_LEN, D), 1.0),
        'c_ctx': nrm((D,), 1.0),
        'ada_w': nrm((L, D, 6 * D), 0.5 * D ** -0.5),
        'ada_b': nrm((L, 6 * D), 0.02),
        'norm_mix_g': gain((L, D)),
        'norm_ffn_g': gain((L, D)),
        'w_in': nrm((L, D, IN_WIDTH), D ** -0.5),
        'diff_lambda': nrm((L, 4, DIFF_HEAD_DIM), 0.1),
        'diff_subln_g': gain((L, DIFF_V_DIM)),
        'hyena_short_w': nrm((L, HYENA_SHORT, HYENA_PROJ), HYENA_SHORT ** -0.5),
        'hyena_short_b': nrm((L, HYENA_PROJ), 0.02),
        'filt_w1': nrm((L, FILT_EMB, FILT_HIDDEN), FILT_EMB ** -0.5),
        'filt_b1': nrm((L, FILT_HIDDEN), 0.02),
        'filt_freq': gain((L, 2, FILT_HIDDEN)),
        'filt_w2': nrm((L, FILT_HIDDEN, FILT_HIDDEN), FILT_HIDDEN ** -0.5),
        'filt_b2': nrm((L, FILT_HIDDEN), 0.02),
        'filt_w3': nrm((L, FILT_HIDDEN, HYENA_ORDER * 2 * HYENA_WIDTH), FILT_HIDDEN ** -0.5),
        'hyena_skip': nrm((L, HYENA_ORDER, HYENA_WIDTH), 0.5),
        'conf_dw_w': nrm((L, CONF_KERNEL, CONF_WIDTH), CONF_KERNEL ** -0.5),
        'conf_ln_g': gain((L, CONF_WIDTH)),
        'conf_ln_b': nrm((L, CONF_WIDTH), 0.02),
        'mla_q_norm_g': gain((L, MLA_Q_RANK)),
        'mla_kv_norm_g': gain((L, MLA_KV_RANK)),
        'mla_w_uq': nrm((L, MLA_Q_RANK, MLA_HEADS * (MLA_NOPE + MLA_ROPE)), MLA_Q_RANK ** -0.5),
        'mla_w_ukv': nrm((L, MLA_KV_RANK, MLA_HEADS * (MLA_NOPE + MLA_V)), MLA_KV_RANK ** -0.5),
        'w_branch': nrm((L, MIX_WIDTH, D), 512 ** -0.5),
        'w_out': nrm((L, D, D), D ** -0.5),
        'w_router': nrm((L, D, N_EXPERTS), D ** -0.5),
        'w_exp_in': nrm((L, N_EXPERTS, D, 2 * EXPERT_HIDDEN), D ** -0.5),
        'w_exp_out': nrm((L, N_EXPERTS, EXPERT_HIDDEN, D), EXPERT_HIDDEN ** -0.5),
        'final_norm_g': gain((D,)),
    }


def reference(x, c, ctx, c_ctx, ada_w, ada_b, norm_mix_g, norm_ffn_g, w_in, diff_lambda, diff_subln_g,
              hyena_short_w, hyena_short_b, filt_w1, filt_b1, filt_freq, filt_w2, filt_b2, filt_w3, hyena_skip,
              conf_dw_w, conf_ln_g, conf_ln_b, mla_q_norm_g, mla_kv_norm_g, mla_w_uq, mla_w_ukv,
              w_branch, w_out, w_router, w_exp_in, w_exp_out, final_norm_g):
    n_lat = x.shape[1]
    rope_diff = axial_rope_tables(n_lat, DIFF_HEAD_DIM)
    rope_mla = axial_rope_tables(n_lat, MLA_ROPE)
    s_lat = jax.nn.silu(c)
    s_ctx = jax.nn.silu(c_ctx)[None]
    h_lat, h_ctx = x, ctx
    for l in range(DEPTH):
        last = l == DEPTH - 1
        p = dict(w_in=w_in[l], diff_subln_g=diff_subln_g[l], hyena_short_w=hyena_short_w[l],
                 hyena_short_b=hyena_short_b[l], filt_w1=filt_w1[l], filt_b1=filt_b1[l], filt_freq=filt_freq[l],
                 filt_w2=filt_w2[l], filt_b2=filt_b2[l], filt_w3=filt_w3[l], hyena_skip=hyena_skip[l],
                 conf_dw_w=conf_dw_w[l], conf_ln_g=conf_ln_g[l], conf_ln_b=conf_ln_b[l],
                 mla_q_norm_g=mla_q_norm_g[l], mla_kv_norm_g=mla_kv_norm_g[l], mla_w_uq=mla_w_uq[l],
                 mla_w_ukv=mla_w_ukv[l], w_branch=w_branch[l], w_out=w_out[l], w_router=w_router[l],
                 w_exp_in=w_exp_in[l], w_exp_out=w_exp_out[l])
        mod_lat = jnp.split((s_lat @ ada_w[l] + ada_b[l])[:, None, :], 6, axis=-1)
        mod_ctx = jnp.split((s_ctx @ ada_w[l] + ada_b[l])[:, None, :], 6, axis=-1)
        lam_init = 0.8 - 0.6 * math.exp(-0.3 * l)
        lq1, lk1, lq2, lk2 = diff_lambda[l].astype(jnp.float32)
        lam = jnp.exp(jnp.sum(lq1 * lk1)) - jnp.exp(jnp.sum(lq2 * lk2)) + lam_init
        u_lat = modulate(rms_norm(h_lat, norm_mix_g[l]), mod_lat[0], mod_lat[1])
        u_ctx = modulate(rms_norm(h_ctx, norm_mix_g[l]), mod_ctx[0], mod_ctx[1])
        y_lat, y_ctx = token_mixer(u_lat, u_ctx, p, lam, lam_init, rope_diff, rope_mla, not last)
        h_lat = h_lat + mod_lat[2] * y_lat
        u_lat = modulate(rms_norm(h_lat, norm_ffn_g[l]), mod_lat[3], mod_lat[4])
        h_lat = h_lat + mod_lat[5] * expert_choice_ffn(u_lat, p)
        if not last:
            h_ctx = h_ctx + mod_ctx[2] * y_ctx
            u_ctx = modulate(rms_norm(h_ctx, norm_ffn_g[l]), mod_ctx[3], mod_ctx[4])
            h_ctx = h_ctx + mod_ctx[5] * expert_choice_ffn(u_ctx, p)
    return rms_norm(h_lat, final_norm_g)
```

```python
import math
import os
import numpy as np
import concourse.bass as bass
import concourse.mybir as mybir
from concourse.bass_utils import run_bass_kernel_spmd

F32 = mybir.dt.float32
BF16 = mybir.dt.bfloat16
I32 = mybir.dt.int32
AF = mybir.ActivationFunctionType
ALU = mybir.AluOpType
AX = mybir.AxisListType

COMPUTE = ("pe", "act", "dve", "pool")


class Buf:
    __slots__ = ("name", "last_w", "readers", "sem_in", "sem_out")

    def __init__(self, name):
        self.name = name
        self.last_w = None
        self.readers = []
        self.sem_in = None
        self.sem_out = None


class Op:
    __slots__ = ("eng", "fn", "deps", "signals", "sem", "count", "is_dma", "ndma", "idx")


class Sched:
    def __init__(self, nc):
        self.nc = nc
        self.ops = []
        self.eng_sem = {}
        self.dma_sems = []
        self.out_ops = []

    def _record(self, eng, fn, reads, writes, is_dma=False, ndma=1, sem_owner=None, acc=False):
        op = Op()
        op.eng = eng
        op.fn = fn
        op.is_dma = is_dma
        op.ndma = ndma
        op.signals = is_dma
        op.sem = None
        op.count = 0
        op.idx = len(self.ops)
        reads = list({id(b): b for b in reads}.values())
        writes = list({id(b): b for b in writes}.values())
        deps = set()
        for b in reads:
            if b.last_w is not None:
                deps.add(b.last_w)
        for b in writes:
            if b.last_w is not None:
                deps.add(b.last_w)
            for r in b.readers:
                deps.add(r)
        deps.discard(op.idx)
        if eng == "pe":
            deps = {d for d in deps if self.ops[d].eng != "pe" or self.ops[d].is_dma}
        op.deps = deps
        for b in writes:
            b.last_w = op.idx
            b.readers = []
        for b in reads:
            if b.last_w != op.idx:
                if not is_dma:
                    b.readers = [r for r in b.readers if self.ops[r].is_dma or self.ops[r].eng != eng]
                b.readers.append(op.idx)
        if is_dma:
            op.sem = sem_owner
        self.ops.append(op)
        return op

    def op(self, eng, fn, reads=(), writes=()):
        return self._record(eng, fn, list(reads), list(writes))

    def dma(self, queue, pairs, reads=(), writes=(), owner=None, kind="in"):
        assert owner is not None
        key = (owner, kind)
        op = self._record(queue, pairs, list(reads), list(writes), is_dma=True,
                          ndma=len(pairs), sem_owner=key)
        return op

    def emit(self):
        nc = self.nc
        ops = self.ops
        for o in ops:
            for d in o.deps:
                ops[d].signals = True
        last_of = {}
        for o in ops:
            last_of[o.eng] = o.idx
        final_deps = set(last_of.values())
        for o in ops:
            if o.is_dma:
                final_deps.add(o.idx)
        for d in final_deps:
            ops[d].signals = True
        sem_objs = {}

        def get_sem(key):
            if key not in sem_objs:
                sem_objs[key] = nc.alloc_semaphore(name="s_%d" % len(sem_objs))
            return sem_objs[key]

        counts = {}
        for o in ops:
            if o.is_dma:
                key = ("dma", id(o.sem[0]), o.sem[1])
                o.sem = key
                counts[key] = counts.get(key, 0) + 16 * o.ndma
                o.count = counts[key]
            elif o.signals:
                key = ("eng", o.eng)
                o.sem = key
                counts[key] = counts.get(key, 0) + 1
                o.count = counts[key]
        self.n_sems = len(set(o.sem for o in ops if o.sem is not None))
        by_eng = {}
        for o in ops:
            by_eng.setdefault(o.eng, []).append(o)
        handles = {"pe": nc.tensor, "act": nc.scalar, "dve": nc.vector, "pool": nc.gpsimd, "sp": nc.sync}
        final_waits = {}
        for d in final_deps:
            od = ops[d]
            final_waits[od.sem] = max(final_waits.get(od.sem, 0), od.count)
        self.n_waits = 0

        def run_engine(ename, h):
            known = {}
            for o in by_eng.get(ename, []):
                need = {}
                for d in o.deps:
                    od = ops[d]
                    need[od.sem] = max(need.get(od.sem, 0), od.count)
                for key, v in need.items():
                    if known.get(key, 0) >= v:
                        continue
                    h.wait_ge(get_sem(key), v)
                    known[key] = v
                    self.n_waits += 1
                if o.is_dma:
                    s = get_sem(o.sem)
                    for (oap, iap) in o.fn:
                        h.dma_start(out=oap, in_=iap).then_inc(s, 16)
                else:
                    ins = o.fn(h)
                    if o.signals:
                        ins.then_inc(get_sem(o.sem), 1)
            if ename == "sp":
                for key, v in final_waits.items():
                    if known.get(key, 0) >= v:
                        continue
                    h.wait_ge(get_sem(key), v)

        with nc.Block() as block:
            @block.sync
            def _(e):
                run_engine("sp", e)

            @block.tensor
            def _(e):
                run_engine("pe", e)

            @block.scalar
            def _(e):
                run_engine("act", e)

            @block.vector
            def _(e):
                run_engine("dve", e)

            @block.gpsimd
            def _(e):
                run_engine("pool", e)


import contextlib


class View:
    __slots__ = ("buf", "ap")

    def __init__(self, buf, ap):
        self.buf = buf
        self.ap = ap


class Tile:
    def __init__(self, t, name):
        self.t = t
        self.buf = Buf(name)

    def __getitem__(self, key):
        return View(self.buf, self.t[key])

    def v(self, ap):
        return View(self.buf, ap)


def _ap(x):
    return x.ap if isinstance(x, View) else x


def _bufs(*xs):
    return [x.buf for x in xs if isinstance(x, View)]


class KB:
    def __init__(self):
        self.nc = bass.Bass("TRN2", target_bir_lowering=False)
        self.S = Sched(self.nc)
        self.st = contextlib.ExitStack()
        self.n = 0

    def sb(self, shape, dt, name=None):
        self.n += 1
        name = name or "sb%d" % self.n
        return Tile(self.st.enter_context(self.nc.sbuf_tensor(name, list(shape), dt)), name)

    def ps(self, shape, dt, name=None):
        self.n += 1
        name = name or "ps%d" % self.n
        return Tile(self.st.enter_context(self.nc.psum_tensor(name, list(shape), dt)), name)

    def dram(self, name, shape, dt, kind):
        return Tile(self.nc.dram_tensor(name, list(shape), dt, kind=kind), "d_" + name)

    def dma(self, q, out, in_):
        sbuf_side = in_ if out.buf.name.startswith("d_") else out
        kind = "in" if sbuf_side is out else "out"
        self.S.dma(q, [(out.ap, in_.ap)], reads=[in_.buf], writes=[out.buf], owner=sbuf_side.buf, kind=kind)

    def mm(self, out, lhsT, rhs, start=True, stop=True):
        o, l, r = out.ap, lhsT.ap, rhs.ap
        self.S.op("pe", lambda e: e.matmul(o, lhsT=l, rhs=r, start=start, stop=stop),
                  reads=[lhsT.buf, rhs.buf], writes=[out.buf])

    def transpose(self, out, in_, ident):
        o, i, d = out.ap, in_.ap, ident.ap
        self.S.op("pe", lambda e: e.transpose(o, i, d), reads=[in_.buf, ident.buf], writes=[out.buf])

    def act(self, out, in_, func, bias=None, scale=None, accum=None):
        kw = {}
        if bias is not None:
            kw["bias"] = _ap(bias)
        if scale is not None:
            kw["scale"] = _ap(scale)
        if accum is not None:
            kw["accum_out"] = accum.ap
        o, i = out.ap, in_.ap
        self.S.op("act", lambda e: e.activation(out=o, in_=i, func=func, **kw),
                  reads=[in_.buf] + _bufs(bias, scale), writes=[out.buf] + _bufs(accum))

    def tt(self, eng, out, in0, in1, op):
        o, a, b = out.ap, in0.ap, in1.ap
        self.S.op(eng, lambda e: e.tensor_tensor(out=o, in0=a, in1=b, op=op),
                  reads=[in0.buf, in1.buf], writes=[out.buf])

    def ts(self, eng, out, in0, s1, op0, s2=None, op1=None, accum=None):
        o, a = out.ap, in0.ap
        kw = {}
        if op1 is not None:
            kw["op1"] = op1
        if accum is not None:
            kw["accum_out"] = accum.ap
        s1a, s2a = _ap(s1), _ap(s2)
        self.S.op(eng, lambda e: e.tensor_scalar(out=o, in0=a, scalar1=s1a, scalar2=s2a, op0=op0, **kw),
                  reads=[in0.buf] + _bufs(s1, s2), writes=[out.buf] + _bufs(accum))

    def stt(self, eng, out, in0, scalar, in1, op0, op1):
        o, a, b = out.ap, in0.ap, in1.ap
        sa = _ap(scalar)
        self.S.op(eng, lambda e: e.scalar_tensor_tensor(out=o, in0=a, scalar=sa, in1=b, op0=op0, op1=op1),
                  reads=[in0.buf, in1.buf] + _bufs(scalar), writes=[out.buf])

    def copy(self, eng, out, in_):
        o, i = out.ap, in_.ap
        if eng == "act":
            self.S.op("act", lambda e: e.copy(out=o, in_=i), reads=[in_.buf], writes=[out.buf])
        else:
            self.S.op(eng, lambda e: e.tensor_copy(out=o, in_=i), reads=[in_.buf], writes=[out.buf])

    def recip(self, out, in_):
        o, i = out.ap, in_.ap
        self.S.op("dve", lambda e: e.reciprocal(out=o, in_=i), reads=[in_.buf], writes=[out.buf])

    def memset(self, eng, out, val):
        o = out.ap
        self.S.op(eng, lambda e: e.memset(o, val), writes=[out.buf])

    def reduce(self, eng, out, in_, op, axis=None):
        o, i = out.ap, in_.ap
        ax = axis or AX.X
        self.S.op(eng, lambda e: e.tensor_reduce(out=o, in_=i, axis=ax, op=op), reads=[in_.buf], writes=[out.buf])

    def finish(self):
        self.S.emit()
        self.st.close()
        return self.nc


D = 1024
KC = 8
L = 2


def build_P0():
    k = KB()
    cvec = k.dram("cvec", [128, KC * 3], F32, "ExternalInput")
    ada_w = k.dram("ada_w", [L, D, 6 * D], F32, "ExternalInput")
    ada_b = k.dram("ada_b", [L, 128, 48], F32, "ExternalInput")
    mods = k.dram("mods", [L, 128, 48 * 3], F32, "ExternalOutput")
    cv = k.sb([128, KC, 3], F32)
    k.dma("sp", cv[:], cvec.v(cvec.t.rearrange("p (c j) -> p c j", j=3)))
    s = k.sb([128, KC, 3], F32)
    k.act(s[:], cv[:], AF.Silu)
    wst = [k.sb([128, KC, 512], F32, "wst%d" % i) for i in range(2)]
    ps = [k.ps([128, 512], F32, "ps%d" % i) for i in range(2)]
    it = 0
    for l in range(L):
        bt = k.sb([128, 48], F32, "bt%d" % l)
        k.dma("sp", bt[:], ada_b[l, :, :])
        mo = k.sb([128, 48, 3], F32, "mo%d" % l)
        wv = ada_w.t[l].rearrange("(c p) f -> p c f", p=128)
        for cg in range(12):
            w = wst[cg % 2]
            k.dma("sp", w[:], ada_w.v(wv[:, :, cg * 512:(cg + 1) * 512]))
            for f in range(4):
                fi = cg * 4 + f
                p = ps[it % 2]; it += 1
                for c in range(KC):
                    k.mm(p[:, 0:3], w[:, c, f * 128:(f + 1) * 128], s[:, c, :], start=(c == 0), stop=(c == KC - 1))
                k.ts("dve", mo[:, fi, :], p[:, 0:3], bt[:, fi:fi + 1], ALU.add)
        k.dma("sp", mods.v(mods.t[l].rearrange("p (f j) -> p f j", j=3)), mo[:])
    return k.finish()


def lay(v):
    v = np.asarray(v, np.float32).reshape(-1, 128)
    return np.ascontiguousarray(v.T)


D = 1024
KC = 8
EPS = 1e-6
NA = 3072


def build_A(T):
    k = KB()
    TW = min(512, T)
    NT = T // TW
    hT = k.dram("hT", [D, T], F32, "ExternalInput")
    mods = k.dram("mods", [128, 48], F32, "ExternalInput")
    gmix = k.dram("gmix", [128, KC], F32, "ExternalInput")
    wA = k.dram("wA", [D, NA], F32, "ExternalInput")
    wv = k.dram("wv", [D, 512], F32, "ExternalInput")
    cosd = k.dram("cosd", [128, T], F32, "ExternalInput"); sind = k.dram("sind", [128, T], F32, "ExternalInput")
    cosq = k.dram("cosq", [96, T], F32, "ExternalInput"); sinq = k.dram("sinq", [96, T], F32, "ExternalInput")
    perm = k.dram("perm", [128, 128], F32, "ExternalInput")
    permq = k.dram("permq", [96, 96], F32, "ExternalInput")
    perm32 = k.dram("perm32", [32, 32], F32, "ExternalInput")
    gq = k.dram("gq", [128, 2], F32, "ExternalInput")
    gkv = k.dram("gkv", [128, 1], F32, "ExternalInput")
    wuq = k.dram("wuq", [256, 384], F32, "ExternalInput")
    wuk = k.dram("wuk", [128, 256], F32, "ExternalInput")
    wuv = k.dram("wuv", [128, 256], F32, "ExternalInput")
    qkT = k.dram("qkT", [8, 128, T], BF16, "ExternalOutput")
    vtok = k.dram("vtok", [T, 512], BF16, "ExternalOutput")
    hyT = k.dram("hyT", [768, T], F32, "ExternalOutput")
    gluT = k.dram("gluT", [256, T], F32, "ExternalOutput")
    mqT = k.dram("mqT", [4, 96, T], BF16, "ExternalOutput")
    mkT = k.dram("mkT", [4, 96, T], BF16, "ExternalOutput")
    mvtok = k.dram("mvtok", [T, 256], BF16, "ExternalOutput")

    ones = k.sb([128, 128], F32); k.memset("dve", ones[:], 1.0)
    modt = k.sb([128, 48], F32); k.dma("sp", modt[:], mods[:, :])
    gm = k.sb([128, KC], F32); k.dma("sp", gm[:], gmix[:, :])
    Acoef = k.sb([128, KC], F32)
    k.ts("dve", Acoef[:], modt[:, 8:16], 1.0, ALU.add)
    k.tt("dve", Acoef[:], Acoef[:], gm[:], ALU.mult)

    def load_bf(dr, shape, view=None):
        f = k.sb(shape, F32); b = k.sb(shape, BF16)
        k.dma("pool", f[:], dr[:, :] if view is None else dr.v(view))
        k.copy("dve", b[:], f[:])
        return b
    permb = load_bf(perm, [128, 128]); permqb = load_bf(permq, [96, 96])
    perm32b = load_bf(perm32, [32, 32])
    wuqb = load_bf(wuq, [128, 2, 384], wuq.t.rearrange("(j p) f -> p j f", p=128))
    wukb = load_bf(wuk, [128, 256]); wuvb = load_bf(wuv, [128, 256])
    gqs = k.sb([128, 2], F32); k.dma("sp", gqs[:], gq[:, :])
    gkvs = k.sb([128, 1], F32); k.dma("sp", gkvs[:], gkv[:, :])

    uT = k.sb([128, KC, T], BF16, "uT")
    hT_v = hT.t.rearrange("(c p) t -> p c t", p=128)
    ps_ssq = k.ps([128, TW], F32)
    ht = k.sb([128, KC, TW], F32, "ht"); sq = k.sb([128, KC, TW], F32, "sq")
    rstd = k.sb([128, TW], F32, "rstd"); tmp = k.sb([128, TW], F32, "tmpn")

    def rstd_from(ps, n_feat):
        k.ts("dve", rstd[:], ps[:], 1.0 / n_feat, ALU.mult, EPS, ALU.add)
        k.act(rstd[:], rstd[:], AF.Sqrt)
        k.recip(rstd[:], rstd[:])

    for ti in range(NT):
        ts_ = slice(ti * TW, (ti + 1) * TW)
        k.dma("sp", ht[:], hT.v(hT_v[:, :, ts_]))
        k.act(sq[:], ht[:], AF.Square)
        for c in range(KC):
            k.mm(ps_ssq[:], ones[:], sq[:, c, :], start=(c == 0), stop=(c == KC - 1))
        rstd_from(ps_ssq, D)
        for c in range(KC):
            k.tt("dve", tmp[:], ht[:, c, :], rstd[:], ALU.mult)
            k.ts("dve", uT[:, c, ts_], tmp[:], Acoef[:, c:c + 1], ALU.mult, modt[:, c:c + 1], ALU.add)

    wst = [k.sb([128, KC, 512], F32, "wst%d" % i) for i in range(2)]
    wbf = [k.sb([128, KC, 512], BF16, "wbf%d" % i) for i in range(2)]
    pz = [k.ps([128, TW], F32, "pz%d" % i) for i in range(3)]
    pr = [k.ps([128, TW], F32, "pr%d" % i) for i in range(2)]
    cst = [k.sb([128, TW], F32, "cs%d" % i) for i in range(2)]
    snt = [k.sb([128, TW], F32, "sn%d" % i) for i in range(2)]
    zb = [k.sb([128, TW], BF16, "zb%d" % i) for i in range(2)]
    t1 = [k.sb([128, TW], F32, "t1%d" % i) for i in range(2)]
    t2 = [k.sb([128, TW], F32, "t2%d" % i) for i in range(2)]
    ob = [k.sb([128, TW], BF16, "ob%d" % i) for i in range(2)]
    of = [k.sb([128, TW], F32, "of%d" % i) for i in range(2)]
    cqs = k.sb([128, 2, TW], F32, "cqs"); cqn = k.sb([128, 2, TW], BF16, "cqn")
    ckn = k.sb([128, TW], BF16, "ckn")
    st = {"i": 0, "z": 0}
    wA_v = wA.t.rearrange("(c p) f -> p c f", p=128)

    def proj(wb, f, ts_):
        p = pz[st["z"] % 3]; st["z"] += 1
        for c in range(KC):
            k.mm(p[:], wb[:, c, f * 128:(f + 1) * 128], uT[:, c, ts_], start=(c == 0), stop=(c == KC - 1))
        return p

    def rope(pview, R, cos_d, sin_d, pm, ts_, outs):
        b = st["i"] % 2; st["i"] += 1
        k.dma("sp", cst[b][:R, :], cos_d[:, ts_])
        k.dma("sp", snt[b][:R, :], sin_d[:, ts_])
        k.copy("act", t1[b][:R, :], pview)
        k.copy("dve", zb[b][:R, :], t1[b][:R, :])
        k.mm(pr[b][:R, :], pm[:R, :R], zb[b][:R, :])
        k.copy("act", t2[b][:R, :], pr[b][:R, :])
        k.tt("dve", t1[b][:R, :], t1[b][:R, :], cst[b][:R, :], ALU.mult)
        k.tt("dve", t2[b][:R, :], t2[b][:R, :], snt[b][:R, :], ALU.mult)
        k.tt("dve", ob[b][:R, :], t1[b][:R, :], t2[b][:R, :], ALU.add)
        for o in outs:
            k.dma("sp", o, ob[b][:R, :])

    def store_f32(pview, R, dst):
        b = st["i"] % 2; st["i"] += 1
        k.copy("act", of[b][:R, :], pview)
        k.dma("sp", dst, of[b][:R, :])

    for g in range(6):
        wb = wbf[g % 2]
        k.dma("pool", wst[g % 2][:], wA.v(wA_v[:, :, g * 512:(g + 1) * 512]))
        k.copy("dve", wb[:], wst[g % 2][:])
        for ti in range(NT):
            ts_ = slice(ti * TW, (ti + 1) * TW)
            if g < 2:
                for hd in range(4):
                    p = proj(wb, hd, ts_)
                    rope(p[:], 128, cosd, sind, permb, ts_, [qkT[g * 4 + hd, :, ts_]])
            elif g == 2:
                for f in range(4):
                    p = proj(wb, f, ts_)
                    store_f32(p[:], 128, hyT[f * 128:(f + 1) * 128, ts_])
            elif g == 3:
                for f in range(2):
                    p = proj(wb, f, ts_)
                    store_f32(p[:], 128, hyT[512 + f * 128:512 + (f + 1) * 128, ts_])
                for j in range(2):
                    p = proj(wb, 2 + j, ts_)
                    k.copy("act", cqs[:, j, :], p[:])
                k.act(sq[:, 0:2, :], cqs[:], AF.Square)
                for j in range(2):
                    k.mm(ps_ssq[:], ones[:], sq[:, j, :], start=(j == 0), stop=(j == 1))
                rstd_from(ps_ssq, 256)
                for j in range(2):
                    k.stt("dve", cqn[:, j, :], cqs[:, j, :], gqs[:, j:j + 1], rstd[:], ALU.mult, ALU.mult)
                for h in range(4):
                    p = pz[st["z"] % 3]; st["z"] += 1
                    for j in range(2):
                        k.mm(p[:96, :], wuqb[:, j, h * 96:(h + 1) * 96], cqn[:, j, :], start=(j == 0), stop=(j == 1))
                    rope(p[:96, :], 96, cosq, sinq, permqb, ts_, [mqT[h, :, ts_]])
            elif g == 4:
                for j in range(2):
                    pa = proj(wb, j, ts_)
                    pb = proj(wb, 2 + j, ts_)
                    b = st["i"] % 2; st["i"] += 1
                    k.act(t1[b][:], pb[:], AF.Sigmoid)
                    k.copy("act", t2[b][:], pa[:])
                    k.tt("dve", of[b][:], t1[b][:], t2[b][:], ALU.mult)
                    k.dma("sp", gluT[j * 128:(j + 1) * 128, ts_], of[b][:])
            else:
                p = proj(wb, 0, ts_)
                k.copy("act", cqs[:, 0, :], p[:])
                k.act(sq[:, 0, :], cqs[:, 0, :], AF.Square)
                k.mm(ps_ssq[:], ones[:], sq[:, 0, :])
                rstd_from(ps_ssq, 128)
                k.stt("dve", ckn[:], cqs[:, 0, :], gkvs[:, 0:1], rstd[:], ALU.mult, ALU.mult)
                pk = proj(wb, 1, ts_)
                b = st["i"] % 2; st["i"] += 1
                k.dma("sp", cst[b][:32, :], cosq[64:96, ts_])
                k.dma("sp", snt[b][:32, :], sinq[64:96, ts_])
                k.copy("act", t1[b][:32, :], pk[:32, :])
                k.copy("dve", zb[b][:32, :], t1[b][:32, :])
                k.mm(pr[b][:32, :], perm32b[:, :], zb[b][:32, :])
                k.copy("act", t2[b][:32, :], pr[b][:32, :])
                k.tt("dve", t1[b][:32, :], t1[b][:32, :], cst[b][:32, :], ALU.mult)
                k.tt("dve", t2[b][:32, :], t2[b][:32, :], snt[b][:32, :], ALU.mult)
                k.tt("dve", ob[b][:32, :], t1[b][:32, :], t2[b][:32, :], ALU.add)
                for h in range(4):
                    k.dma("sp", mkT[h, 64:96, ts_], ob[b][:32, :])
                for h in range(4):
                    p = pz[st["z"] % 3]; st["z"] += 1
                    k.mm(p[:64, :], wukb[:, h * 64:(h + 1) * 64], ckn[:])
                    b = st["i"] % 2; st["i"] += 1
                    k.copy("act", ob[b][:64, :], p[:64, :])
                    k.dma("sp", mkT[h, 0:64, ts_], ob[b][:64, :])
                for s in range(TW // 128):
                    p = pz[st["z"] % 3]; st["z"] += 1
                    k.mm(p[:, 0:256], ckn[:, s * 128:(s + 1) * 128], wuvb[:])
                    b = st["i"] % 2; st["i"] += 1
                    k.copy("act", ob[b][:, 0:256], p[:, 0:256])
                    k.dma("sp", mvtok[ti * TW + s * 128: ti * TW + (s + 1) * 128, :], ob[b][:, 0:256])
    k.dma("pool", wst[0][:], wv.v(wv.t.rearrange("(c p) f -> p c f", p=128)))
    k.copy("dve", wbf[0][:], wst[0][:])
    ov = [k.sb([128, 512], BF16, "ov%d" % i) for i in range(2)]
    pvv = [k.ps([128, 512], F32, "pvv%d" % i) for i in range(2)] if TW < 512 else pz
    for tb in range(T // 128):
        b = tb % 2
        for c in range(KC):
            k.mm(pvv[b][:], uT[:, c, tb * 128:(tb + 1) * 128], wbf[0][:, c, :], start=(c == 0), stop=(c == KC - 1))
        k.copy("act", ov[b][:], pvv[b][:])
        k.dma("sp", vtok[tb * 128:(tb + 1) * 128, :], ov[b][:])
    return k.finish()


GRID_W = 64


def lay(v):
    v = np.asarray(v, np.float32).reshape(-1, 128)
    return np.ascontiguousarray(v.T)


def rope_consts(pos, use_rope=True):
    T = len(pos)
    row = (pos // GRID_W).astype(np.float32); col = (pos % GRID_W).astype(np.float32)
    def ang(nf):
        inv = (10000.0 ** (-np.arange(nf, dtype=np.float32) / nf)).astype(np.float32)
        return np.concatenate([row[:, None] * inv, col[:, None] * inv], -1)
    a16, a8 = ang(16), ang(8)
    cosd = np.ones((128, T), np.float32); sind = np.zeros((128, T), np.float32)
    cosq = np.ones((96, T), np.float32); sinq = np.zeros((96, T), np.float32)
    if use_rope:
        for r in range(128):
            j = r % 32
            cosd[r] = np.cos(a16[:, j]); sind[r] = np.sin(a16[:, j]) * (-1.0 if (r % 64) < 32 else 1.0)
        for r in range(32):
            j = r % 16
            cosq[64 + r] = np.cos(a8[:, j]); sinq[64 + r] = np.sin(a8[:, j]) * (-1.0 if r < 16 else 1.0)
    perm = np.zeros((128, 128), np.float32)
    for m in range(128):
        perm[m + 32 if (m % 64) < 32 else m - 32, m] = 1.0
    permq = np.zeros((96, 96), np.float32)
    for m in range(96):
        if m < 64:
            permq[m, m] = 1.0
        else:
            r = m - 64
            permq[64 + (r + 16 if r < 16 else r - 16), m] = 1.0
    perm32 = np.zeros((32, 32), np.float32)
    for r in range(32):
        perm32[r + 16 if r < 16 else r - 16, r] = 1.0
    return dict(cosd=cosd, sind=sind, cosq=cosq, sinq=sinq, perm=perm, permq=permq, perm32=perm32)


def A_weights(w_in, wuq, wukv, gq, gkv, gmix):
    dq, dk, dv = w_in[:, 0:512], w_in[:, 512:1024], w_in[:, 1024:1536]
    hy = w_in[:, 1536:2304]; cf = w_in[:, 2304:2816]; cq = w_in[:, 2816:3072]; ckv = w_in[:, 3072:3200]; kr = w_in[:, 3200:3232]
    pad = np.zeros((1024, 512 - 160), np.float32)
    wA = np.concatenate([dq, dk, hy[:, :512], hy[:, 512:], cq, cf, ckv, kr, pad], 1)
    kcols = np.concatenate([np.arange(h * 128, h * 128 + 64) for h in range(4)])
    vcols = kcols + 64
    return dict(wA=np.ascontiguousarray(wA, np.float32), wv=np.ascontiguousarray(dv, np.float32), wuq=np.ascontiguousarray(wuq, np.float32),
                wuk=np.ascontiguousarray(wukv[:, kcols], np.float32), wuv=np.ascontiguousarray(wukv[:, vcols], np.float32),
                gq=lay(gq), gkv=lay(gkv), gmix=lay(gmix))


def build_attn(nmap, dqk, dv, Tq, Tk, scale, LA=2):
    k = KB()
    QT = k.dram("QT", [nmap, dqk, Tq], BF16, "ExternalInput")
    KT = k.dram("KT", [nmap, dqk, Tk], BF16, "ExternalInput")
    V = k.dram("V", [nmap, Tk, dv], BF16, "ExternalInput")
    OT = k.dram("OT", [nmap, dv, Tq], F32, "ExternalOutput")
    NK = Tk // 128
    QW = min(512, Tq)
    NQ = Tq // QW
    NS = LA + 1
    onesf = k.sb([128, 128], F32); k.memset("dve", onesf[:], 1.0)
    ones = k.sb([128, 128], BF16); k.copy("dve", ones[:], onesf[:])
    kts = [k.sb([dqk, Tk], BF16, "kt%d" % i) for i in range(2)]
    vs = [k.sb([128, NK, dv], BF16, "v%d" % i) for i in range(2)]
    qs = [k.sb([dqk, QW], BF16, "q%d" % i) for i in range(2)]
    S = [k.ps([128, QW], F32, "S%d" % i) for i in range(NS)]
    P = [k.sb([128, QW], BF16, "P%d" % i) for i in range(NS)]
    O = [k.ps([dv, QW], F32, "O%d" % i) for i in range(2)]
    Sm = [k.ps([dv, QW], F32, "Sm%d" % i) for i in range(2)]
    osb = [k.sb([dv, QW], F32, "osb%d" % i) for i in range(2)]
    ssb = [k.sb([dv, QW], F32, "ssb%d" % i) for i in range(2)]
    accP = [k.sb([128, QW], F32, "accP%d" % i) for i in range(2)]
    steps = [(m, qb, t) for m in range(nmap) for qb in range(NQ) for t in range(NK)]

    def issue_qk(i):
        m, qb, t = steps[i]
        blk = m * NQ + qb
        if t == 0:
            if qb == 0:
                k.dma("sp", kts[m % 2][:], KT[m, :, :])
                vv_ = V.t[m].rearrange("(t p) d -> p t d", p=128)
                for t0 in range(0, NK, 64):
                    t1_ = min(NK, t0 + 64)
                    k.dma("pool", vs[m % 2][:, t0:t1_, :], V.v(vv_[:, t0:t1_, :]))
            k.dma("sp", qs[blk % 2][:], QT[m, :, qb * QW:(qb + 1) * QW])
        k.mm(S[i % NS][:], kts[m % 2][:, t * 128:(t + 1) * 128], qs[blk % 2][:])

    for i in range(min(LA, len(steps))):
        issue_qk(i)
    for i, (m, qb, t) in enumerate(steps):
        if i + LA < len(steps):
            issue_qk(i + LA)
        blk = m * NQ + qb
        b = i % NS
        o_ = O[blk % 2]; sm_ = Sm[blk % 2]
        k.act(P[b][:], S[b][:], AF.Exp, scale=scale)
        k.mm(o_[:], vs[m % 2][:, t, :], P[b][:], start=(t == 0), stop=(t == NK - 1))
        if t == 0:
            k.copy("dve", accP[blk % 2][:], P[b][:])
        else:
            k.tt("dve", accP[blk % 2][:], accP[blk % 2][:], P[b][:], ALU.add)
        if t == NK - 1:
            k.mm(sm_[:], onesf[:, :dv], accP[blk % 2][:])
            ob_ = osb[blk % 2]; sb_ = ssb[blk % 2]
            k.copy("act", sb_[:], sm_[:])
            k.recip(sb_[:], sb_[:])
            k.copy("act", ob_[:], o_[:])
            k.tt("dve", ob_[:], ob_[:], sb_[:], ALU.mult)
            k.dma("sp", OT[m, :, qb * QW:(qb + 1) * QW], ob_[:])
    return k.finish()


import math

CPC = 32


def build_HS(R, n, CH=2048):
    k = KB()
    zp = k.dram("zp", [R, n + 2], F32, "ExternalInput")
    wsb = k.dram("wsb", [R, 4], F32, "ExternalInput")
    zo = k.dram("zo", [R, n], F32, "ExternalOutput")
    CH = min(CH, n)
    r0 = 0
    i = 0
    while r0 < R:
        rr = min(128, R - r0)
        w = k.sb([rr, 4], F32, "w%d" % r0)
        k.dma("sp", w[:], wsb[r0:r0 + rr, :])
        for c0 in range(0, n, CH):
            zt = k.sb([128, CH + 2], F32, "zt%d" % (i % 2)) if i < 2 else zts[i % 2]
            ot = k.sb([128, CH], F32, "ot%d" % (i % 2)) if i < 2 else ots[i % 2]
            if i == 0:
                zts, ots = [zt], [ot]
            elif i == 1:
                zts.append(zt); ots.append(ot)
            i += 1
            k.dma("sp", zt[:rr, :], zp[r0:r0 + rr, c0:c0 + CH + 2])
            k.ts("dve", ot[:rr, :], zt[:rr, 0:CH], w[:, 0:1], ALU.mult, w[:, 3:4], ALU.add)
            k.stt("dve", ot[:rr, :], zt[:rr, 1:CH + 1], w[:, 1:2], ot[:rr, :], ALU.mult, ALU.add)
            k.stt("dve", ot[:rr, :], zt[:rr, 2:CH + 2], w[:, 2:3], ot[:rr, :], ALU.mult, ALU.add)
            k.dma("sp", zo[r0:r0 + rr, c0:c0 + CH], ot[:rr, :])
        r0 += rr
    return k.finish()


def build_HF(n):
    k = KB()
    TW = min(512, n)
    NT = n // TW
    zT = k.dram("zT", [2, 33, n], F32, "ExternalInput")
    w1 = k.dram("w1", [33, 64], F32, "ExternalInput")
    w2 = k.dram("w2", [64, 64], F32, "ExternalInput")
    w3 = k.dram("w3", [2, 64, 2 * CPC], F32, "ExternalInput")
    pv = k.dram("pv", [64, 4], F32, "ExternalInput")
    dec = k.dram("dec", [2, 2 * CPC, n], F32, "ExternalInput")
    G = k.dram("G", [2, CPC, 2 * n], BF16, "ExternalOutput")
    w1s = k.sb([33, 64], F32); k.dma("sp", w1s[:], w1[:, :])
    w2s = k.sb([64, 64], F32); k.dma("sp", w2s[:], w2[:, :])
    w3s = k.sb([64, 2, 2 * CPC], F32); k.dma("sp", w3s[:], w3.v(w3.t.rearrange("d k m -> k d m")))
    pvs = k.sb([64, 4], F32); k.dma("sp", pvs[:], pv[:, :])
    H = [k.sb([2 * CPC, n], F32, "H%d" % d) for d in range(2)]
    acc = k.sb([2 * CPC, 2, NT], F32); k.memset("dve", acc[:], 0.0)
    p1 = k.ps([64, TW], F32); p2 = k.ps([64, TW], F32); p3 = k.ps([2 * CPC, TW], F32)
    zt = [k.sb([33, TW], F32, "zt%d" % i) for i in range(2)]
    dt_ = [k.sb([2 * CPC, TW], F32, "dt%d" % i) for i in range(2)]
    x = k.sb([64, TW], F32); yi = k.sb([64, TW], I32); yf = k.sb([64, TW], F32); gg = k.sb([64, TW], F32)
    hid = k.sb([64, TW], F32); hid2 = k.sb([64, TW], F32); junk = k.sb([2 * CPC, TW], F32)

    def sin_layer(out, ps, bcol, fcol):
        k.ts("dve", x[:], ps[:], pvs[:, bcol:bcol + 1], ALU.add, pvs[:, fcol:fcol + 1], ALU.mult)
        k.ts("dve", x[:], x[:], 1.0 / (2 * math.pi), ALU.mult, 8.0, ALU.add)
        k.copy("dve", yi[:], x[:])
        k.copy("dve", yf[:], yi[:])
        k.tt("dve", x[:], x[:], yf[:], ALU.subtract)
        k.ts("dve", gg[:], x[:], 0.5, ALU.is_gt)
        k.tt("dve", x[:], x[:], gg[:], ALU.subtract)
        k.act(out[:], x[:], AF.Sin, scale=2 * math.pi)

    it = 0
    for d in range(2):
        for t in range(NT):
            sl = slice(t * TW, (t + 1) * TW)
            b = it % 2; it += 1
            k.dma("sp", zt[b][:], zT[d, :, sl])
            k.dma("sp", dt_[b][:], dec[d, :, sl])
            k.mm(p1[:], w1s[:], zt[b][:])
            sin_layer(hid, p1, 0, 1)
            k.mm(p2[:], w2s[:], hid[:])
            sin_layer(hid2, p2, 2, 3)
            k.mm(p3[:], w3s[:, d, :], hid2[:])
            k.copy("act", H[d][:, sl], p3[:])
            k.tt("dve", H[d][:, sl], H[d][:, sl], dt_[b][:], ALU.mult)
            k.act(junk[:], H[d][:, sl], AF.Abs, accum=acc[:, d, t:t + 1])
    tot = k.sb([2 * CPC, 2], F32)
    k.reduce("dve", tot[:], acc[:], ALU.add)
    k.recip(tot[:], tot[:])
    ob = [k.sb([2 * CPC, TW], BF16, "ob%d" % i) for i in range(2)]
    it = 0
    for d in range(2):
        for t in range(NT):
            sl = slice(t * TW, (t + 1) * TW)
            b = it % 2; it += 1
            k.ts("dve", ob[b][:], H[d][:, sl], tot[:, d:d + 1], ALU.mult)
            off = (n if d == 0 else 0) + t * TW
            for o in range(2):
                k.dma("sp", G[o, :, off:off + TW], ob[b][o * CPC:(o + 1) * CPC, :])
    return k.finish()


def build_HC(n, CG=16):
    k = KB()
    nb = n // 128
    CG = min(CG, CPC)
    G = k.dram("G", [CPC, 2 * n], BF16, "ExternalInput")
    Ur = k.dram("Ur", [128, CPC, 2, nb], F32, "ExternalInput")
    Vn = k.dram("Vn", [128, CPC, 2, nb], F32, "ExternalInput")
    X = k.dram("X", [128, CPC, 2, nb], F32, "ExternalInput")
    Dbc = k.dram("Dbc", [128, CPC], F32, "ExternalInput")
    Y = k.dram("Y", [128, CPC, 2, nb], F32, "ExternalOutput")
    dsb = k.sb([128, CPC], F32); k.dma("sp", dsb[:], Dbc[:, :])
    uf = k.sb([128, CG, 2, nb], F32); ub = k.sb([128, CG, 2, nb], BF16)
    vn = k.sb([128, CG, 2, nb], F32); xg = k.sb([128, CG, 2, nb], F32); yo = k.sb([128, CG, 2, nb], F32)
    L = 2 * n - 128
    TT = [k.sb([128, L], BF16, "TT%d" % i) for i in range(2)]
    Yp = [k.ps([128, 2, nb], F32, "Yp%d" % i) for i in range(2)]
    ysb = [k.sb([128, 2, nb], F32, "ysb%d" % i) for i in range(2)]
    deltas = [0] + [d for d in range(-(nb - 1), nb) if d != 0]
    ci = 0
    for g0 in range(0, CPC, CG):
        k.dma("pool", uf[:], Ur[:, g0:g0 + CG, :, :])
        k.copy("dve", ub[:], uf[:])
        k.dma("pool", vn[:], Vn[:, g0:g0 + CG, :, :])
        k.dma("pool", xg[:], X[:, g0:g0 + CG, :, :])
        for cc in range(CG):
            c = g0 + cc
            tt = TT[ci % 2]; yp = Yp[ci % 2]; ys = ysb[ci % 2]; ci += 1
            k.dma("sp", tt[:], G.v(bass.AP(tensor=G.t, offset=c * 2 * n + 1, ap=[[1, 128], [1, L]])))
            for i, dl in enumerate(deltas):
                jj0 = 128 * (dl + nb - 1)
                if dl >= 0:
                    blo, bhi, alo = 0, nb - dl, dl
                else:
                    blo, bhi, alo = -dl, nb, 0
                N = bhi - blo
                k.mm(yp[:, :, alo:alo + N], tt[:, jj0:jj0 + 128], ub[:, cc, :, blo:bhi],
                     start=(i == 0), stop=(i == len(deltas) - 1))
            k.copy("act", ys[:], yp[:])
            k.stt("dve", ys[:], vn[:, cc, :, :], dsb[:, c:c + 1], ys[:], ALU.mult, ALU.add)
            k.tt("dve", yo[:, cc, :, :], ys[:], xg[:, cc, :, :], ALU.mult)
        k.dma("sp", Y[:, g0:g0 + CG, :, :], yo[:])
    return k.finish()


def hyena_consts(n, ch0):
    t = np.linspace(0.0, 1.0, n, dtype=np.float32)[:, None]
    bands = 16
    w = (np.float32(2.0 * math.pi / n) * np.arange(n, dtype=np.float32))[:, None]
    f = np.linspace(1e-4, bands - 1, bands, dtype=np.float32)[None, :]
    z = np.concatenate([t, np.cos(f * w), -np.sin(f * w)], -1).astype(np.float32)
    deltas = np.abs(np.linspace(math.log(1e-2) / 1.5, math.log(1e-2) / 0.3, 256, dtype=np.float32))
    dec = np.exp(-t * deltas[None, :]).astype(np.float32)
    dsel = dec[:, ch0:ch0 + CPC].T
    d2 = np.concatenate([dsel, dsel], 0)
    zT = np.stack([z.T, z[::-1].T]).astype(np.float32)
    dd = np.stack([d2, d2[:, ::-1]]).astype(np.float32)
    return np.ascontiguousarray(zT), np.ascontiguousarray(dd)


def to_blocks(u, rev=False):
    C, B, n = u.shape
    a = u.reshape(C, B, n // 128, 128)
    if rev:
        a = a[..., ::-1]
    return np.ascontiguousarray(a.transpose(3, 0, 1, 2))


def from_blocks(y):
    q, C, B, nb = y.shape
    return np.ascontiguousarray(y.transpose(1, 2, 3, 0).reshape(C, B, nb * 128))


import math

D = 1024
KC = 8
EPS = 1e-6


def build_C(T, lam_init):
    k = KB()
    TW = min(512, T)
    NT = T // TW
    hT = k.dram("hT", [D, T], F32, "ExternalInput")
    mods = k.dram("mods", [128, 48], F32, "ExternalInput")
    gmix = k.dram("gmix", [128, KC], F32, "ExternalInput")
    gffn = k.dram("gffn", [128, KC], F32, "ExternalInput")
    odT = k.dram("odT", [8, 128, T], F32, "ExternalInput")
    dlam = k.dram("dlam", [1, 256], F32, "ExternalInput")
    gsub = k.dram("gsub", [128, 1], F32, "ExternalInput")
    yhT = k.dram("yhT", [256, T], F32, "ExternalInput")
    ymT = k.dram("ymT", [256, T], F32, "ExternalInput")
    glup = k.dram("glup", [256, T + 30], F32, "ExternalInput")
    cw = k.dram("cw", [128, 2 * 31], F32, "ExternalInput")
    lnp = k.dram("lnp", [128, 4], F32, "ExternalInput")
    wg = k.dram("wg", [D, 4096], F32, "ExternalInput")
    wbr = k.dram("wbr", [1280, D], F32, "ExternalInput")
    wo = k.dram("wo", [D, D], F32, "ExternalInput")
    wr = k.dram("wr", [D, 16], F32, "ExternalInput")
    hmidT = k.dram("hmidT", [D, T], F32, "ExternalOutput")
    u2T = k.dram("u2T", [D, T], BF16, "ExternalOutput")
    aff = k.dram("aff", [T, 16], F32, "ExternalOutput")

    ones = k.sb([128, 128], F32); k.memset("dve", ones[:], 1.0)
    modt = k.sb([128, 48], F32); k.dma("sp", modt[:], mods[:, :])
    gm = k.sb([128, KC], F32); k.dma("sp", gm[:], gmix[:, :])
    gf = k.sb([128, KC], F32); k.dma("sp", gf[:], gffn[:, :])
    A1 = k.sb([128, KC], F32); A2 = k.sb([128, KC], F32)
    k.ts("dve", A1[:], modt[:, 8:16], 1.0, ALU.add); k.tt("dve", A1[:], A1[:], gm[:], ALU.mult)
    k.ts("dve", A2[:], modt[:, 32:40], 1.0, ALU.add); k.tt("dve", A2[:], A2[:], gf[:], ALU.mult)
    dl = k.sb([128, 256], F32)
    k.dma("sp", dl[:], dlam.v(bass.AP(tensor=dlam.t, offset=0, ap=[[0, 128], [1, 256]])))
    pr_ = k.sb([128, 2, 64], F32); e12 = k.sb([128, 2], F32); nlam = k.sb([128, 1], F32)
    k.tt("dve", pr_[:, 0, :], dl[:, 0:64], dl[:, 64:128], ALU.mult)
    k.tt("dve", pr_[:, 1, :], dl[:, 128:192], dl[:, 192:256], ALU.mult)
    k.reduce("dve", e12[:], pr_[:], ALU.add)
    k.act(e12[:], e12[:], AF.Exp)
    k.tt("dve", nlam[:], e12[:, 1:2], e12[:, 0:1], ALU.subtract)
    k.ts("dve", nlam[:], nlam[:], -lam_init, ALU.add)
    gs = k.sb([128, 1], F32); k.dma("sp", gs[:], gsub[:, :])
    k.ts("dve", gs[:], gs[:], 1.0 - lam_init, ALU.mult)
    cws = k.sb([128, 2, 31], F32); k.dma("sp", cws[:], cw.v(cw.t.rearrange("p (j t) -> p j t", j=2)))
    lns = k.sb([128, 4], F32); k.dma("sp", lns[:], lnp[:, :])
    wrs = k.sb([128, KC, 16], F32); k.dma("sp", wrs[:], wr.v(wr.t.rearrange("(c p) e -> p c e", p=128)))

    wgb = k.sb([128, KC, 4096], BF16, "wgb"); wbb = k.sb([128, 10, D], BF16, "wbb"); wob = k.sb([128, KC, D], BF16, "wob")
    stg = [k.sb([128, KC, 128], F32, "stg%d" % i) for i in range(2)]
    n = 0
    wg_v = wg.t.rearrange("(c p) f -> p c f", p=128); wo_v = wo.t.rearrange("(c p) f -> p c f", p=128)
    wb_v = wbr.t.rearrange("(c p) f -> p c f", p=128)
    for c0 in range(0, 4096, 128):
        s_ = stg[n % 2]; n += 1
        k.dma("pool", s_[:], wg.v(wg_v[:, :, c0:c0 + 128])); k.copy("dve", wgb[:, :, c0:c0 + 128], s_[:])
    for c0 in range(0, D, 128):
        s_ = stg[n % 2]; n += 1
        k.dma("pool", s_[:], wo.v(wo_v[:, :, c0:c0 + 128])); k.copy("dve", wob[:, :, c0:c0 + 128], s_[:])
    for c0 in range(0, D, 128):
        for r0 in (0, 5):
            s_ = stg[n % 2]; n += 1
            k.dma("pool", s_[:, 0:5, :], wbr.v(wb_v[:, r0:r0 + 5, c0:c0 + 128])); k.copy("dve", wbb[:, r0:r0 + 5, c0:c0 + 128], s_[:, 0:5, :])

    B1 = k.sb([128, KC, TW], F32, "B1"); B2 = k.sb([128, KC, TW], F32, "B2")
    ub = k.sb([128, KC, TW], BF16, "ub"); accb = k.sb([128, KC, TW], BF16, "accb")
    rstd = k.sb([128, TW], F32, "rstd"); tmp = k.sb([128, TW], F32, "tmp")
    o0 = k.sb([128, TW], F32); o1 = k.sb([128, TW], F32)
    ydb = k.sb([128, 4, TW], BF16); yhf = k.sb([128, 2, TW], F32); yhb = k.sb([128, 2, TW], BF16)
    ymf = yhf; ymb = k.sb([128, 2, TW], BF16); ycb = k.sb([128, 2, TW], BF16)
    glt = k.sb([128, TW + 30], F32); ucv = k.sb([128, 2, TW], F32)
    mean = k.sb([128, TW], F32); msq = k.sb([128, TW], F32)
    sg = k.sb([128, TW], F32); pb = k.sb([128, TW], F32); accf = k.sb([128, TW], F32)
    lg = k.sb([128, 16], F32); mx = k.sb([128, 1], F32); sm = k.sb([128, 1], F32); ex = k.sb([128, 16], F32)
    ps1 = k.ps([128, TW], F32); ps2 = k.ps([128, TW], F32)
    pg = [k.ps([128, TW], F32, "pg%d" % i) for i in range(2)]
    pp = [k.ps([128, TW], F32, "pp%d" % i) for i in range(2)]
    pl = k.ps([128, 16], F32)
    hT_v = hT.t.rearrange("(c p) t -> p c t", p=128)
    hm_v = hmidT.t.rearrange("(c p) t -> p c t", p=128)
    u2_v = u2T.t.rearrange("(c p) t -> p c t", p=128)
    ybr = [(ydb, 4, 0), (yhb, 2, 4), (ycb, 2, 6), (ymb, 2, 8)]

    def rstd_from(ps, n_feat):
        k.ts("dve", rstd[:], ps[:], 1.0 / n_feat, ALU.mult, EPS, ALU.add)
        k.act(rstd[:], rstd[:], AF.Sqrt)
        k.recip(rstd[:], rstd[:])

    def norm_mod(src, dst_f, dst_b, A, shift_col):
        k.act(B2[:], src[:], AF.Square)
        for c in range(KC):
            k.mm(ps1[:], ones[:], B2[:, c, :], start=(c == 0), stop=(c == KC - 1))
        rstd_from(ps1, D)
        for c in range(KC):
            k.tt("dve", tmp[:], src[:, c, :], rstd[:], ALU.mult)
            if dst_f is not None:
                k.ts("dve", dst_f[:, c, :], tmp[:], A[:, c:c + 1], ALU.mult, modt[:, shift_col + c:shift_col + c + 1], ALU.add)
                k.copy("dve", dst_b[:, c, :], dst_f[:, c, :])
            else:
                k.ts("dve", dst_b[:, c, :], tmp[:], A[:, c:c + 1], ALU.mult, modt[:, shift_col + c:shift_col + c + 1], ALU.add)

    for ti in range(NT):
        ts_ = slice(ti * TW, (ti + 1) * TW)
        k.dma("sp", B1[:], hT.v(hT_v[:, :, ts_]))
        norm_mod(B1, None, ub, A1, 0)
        for h in range(4):
            k.dma("sp", o0[:], odT[2 * h, :, ts_]); k.dma("sp", o1[:], odT[2 * h + 1, :, ts_])
            k.stt("dve", o0[:], o1[:], nlam[:, 0:1], o0[:], ALU.mult, ALU.add)
            k.act(o1[:], o0[:], AF.Square)
            k.mm(ps1[:], ones[:], o1[:])
            rstd_from(ps1, 128)
            k.stt("dve", ydb[:, h, :], o0[:], gs[:, 0:1], rstd[:], ALU.mult, ALU.mult)
        for j in range(2):
            k.dma("sp", glt[:], glup[j * 128:(j + 1) * 128, ti * TW: ti * TW + TW + 30])
            k.ts("dve", ucv[:, j, :], glt[:, 0:TW], cws[:, j, 0:1], ALU.mult)
            for t in range(1, 31):
                k.stt("dve", ucv[:, j, :], glt[:, t:t + TW], cws[:, j, t:t + 1], ucv[:, j, :], ALU.mult, ALU.add)
        k.act(B2[:, 0:2, :], ucv[:], AF.Square)
        for j in range(2):
            k.mm(ps1[:], ones[:], ucv[:, j, :], start=(j == 0), stop=(j == 1))
        for j in range(2):
            k.mm(ps2[:], ones[:], B2[:, j, :], start=(j == 0), stop=(j == 1))
        k.ts("dve", mean[:], ps1[:], 1.0 / 256, ALU.mult)
        k.tt("dve", msq[:], mean[:], mean[:], ALU.mult)
        k.ts("dve", rstd[:], ps2[:], 1.0 / 256, ALU.mult, EPS, ALU.add)
        k.tt("dve", rstd[:], rstd[:], msq[:], ALU.subtract)
        k.act(rstd[:], rstd[:], AF.Sqrt)
        k.recip(rstd[:], rstd[:])
        for j in range(2):
            k.tt("dve", tmp[:], ucv[:, j, :], mean[:], ALU.subtract)
            k.tt("dve", tmp[:], tmp[:], rstd[:], ALU.mult)
            k.ts("dve", tmp[:], tmp[:], lns[:, j:j + 1], ALU.mult, lns[:, 2 + j:3 + j], ALU.add)
            k.act(ycb[:, j, :], tmp[:], AF.Silu)
        k.dma("sp", yhf[:], yhT.v(yhT.t.rearrange("(j p) t -> p j t", p=128)[:, :, ts_])); k.copy("dve", yhb[:], yhf[:])
        k.dma("sp", ymf[:], ymT.v(ymT.t.rearrange("(j p) t -> p j t", p=128)[:, :, ts_])); k.copy("dve", ymb[:], ymf[:])
        it = 0
        for fc in range(KC):
            for i, (yt, nch, r0) in enumerate(ybr):
                g_ = pg[it % 2]; p_ = pp[it % 2]; it += 1
                for c in range(KC):
                    k.mm(g_[:], wgb[:, c, i * 1024 + fc * 128: i * 1024 + (fc + 1) * 128], ub[:, c, :], start=(c == 0), stop=(c == KC - 1))
                for c in range(nch):
                    k.mm(p_[:], wbb[:, r0 + c, fc * 128:(fc + 1) * 128], yt[:, c, :], start=(c == 0), stop=(c == nch - 1))
                k.act(sg[:], g_[:], AF.Sigmoid)
                k.copy("act", pb[:], p_[:])
                if i == 0:
                    k.tt("dve", accf[:], sg[:], pb[:], ALU.mult)
                else:
                    k.tt("dve", sg[:], sg[:], pb[:], ALU.mult)
                    k.tt("dve", accf[:], accf[:], sg[:], ALU.add)
            k.copy("dve", accb[:, fc, :], accf[:])
        for fo in range(KC):
            g_ = pg[fo % 2]
            for c in range(KC):
                k.mm(g_[:], wob[:, c, fo * 128:(fo + 1) * 128], accb[:, c, :], start=(c == 0), stop=(c == KC - 1))
            k.ts("dve", tmp[:], g_[:], modt[:, 16 + fo:17 + fo], ALU.mult)
            k.tt("dve", B1[:, fo, :], B1[:, fo, :], tmp[:], ALU.add)
        k.dma("sp", hmidT.v(hm_v[:, :, ts_]), B1[:])
        norm_mod(B1, B2, ub, A2, 24)
        k.dma("sp", u2T.v(u2_v[:, :, ts_]), ub[:])
        for s in range(TW // 128):
            for c in range(KC):
                k.mm(pl[:], B2[:, c, s * 128:(s + 1) * 128], wrs[:, c, :], start=(c == 0), stop=(c == KC - 1))
            k.copy("act", lg[:], pl[:])
            k.reduce("dve", mx[:], lg[:], ALU.max)
            k.ts("dve", mx[:], mx[:], -1.0, ALU.mult)
            k.act(ex[:], lg[:], AF.Exp, bias=mx[:, 0:1], accum=sm[:])
            k.recip(sm[:], sm[:])
            k.ts("dve", ex[:], ex[:], sm[:, 0:1], ALU.mult)
            k.dma("sp", aff[ti * TW + s * 128: ti * TW + (s + 1) * 128, :], ex[:])
    return k.finish()


def lay(v):
    v = np.asarray(v, np.float32).reshape(-1, 128)
    return np.ascontiguousarray(v.T)


D = 1024
KC = 8
EPS = 1e-6
NE = 16


def build_D(T, nseq, cap, final, ST=1536, nexp=NE):
    k = KB()
    TW = min(512, T)
    hmidT = k.dram("hmidT", [D, T], F32, "ExternalInput")
    u2T = k.dram("u2T", [D, T], BF16, "ExternalInput")
    affall = k.dram("affall", [128, nseq // 8], F32, "ExternalInput")
    affT = k.dram("affT", [NE, T], F32, "ExternalInput")
    mods = k.dram("mods", [128, 48], F32, "ExternalInput")
    win = k.dram("win", [NE, D, 2048], F32, "ExternalInput")
    wout = k.dram("wout", [NE, D, D], F32, "ExternalInput")
    mblk = k.dram("mblk", [128, 128], F32, "ExternalInput")
    sel = k.dram("sel", [128, NE], F32, "ExternalInput")
    selb = k.dram("selb", [NE, NE * 128], F32, "ExternalInput")
    gfin = k.dram("gfin", [128, KC], F32, "ExternalInput")
    houtT = k.dram("houtT", [D, T], F32, "ExternalOutput")

    modt = k.sb([128, 48], F32); k.dma("sp", modt[:], mods[:, :])
    mb = k.sb([128, 128], F32); k.dma("sp", mb[:], mblk[:, :])
    sl = k.sb([128, NE], F32); k.dma("sp", sl[:], sel[:, :])
    slb = k.sb([NE, NE, 128], F32); k.dma("sp", slb[:], selb.v(selb.t.rearrange("a (e m) -> a e m", e=NE)))
    gfs = k.sb([128, KC], F32); k.dma("sp", gfs[:], gfin[:, :])
    ones = k.sb([128, 128], F32); k.memset("dve", ones[:], 1.0)
    W = nseq // 8
    aa = k.sb([128, W], F32); k.dma("sp", aa[:], affall[:, :])
    cmpb = k.sb([128, W], F32)
    lo = k.sb([128, 1], F32); hi = k.sb([128, 1], F32); mid = k.sb([128, 1], F32)
    cnt = k.sb([128, 1], F32); cc = k.sb([128, 1], F32); d1 = k.sb([128, 1], F32)
    k.memset("dve", lo[:], 0.0); k.memset("dve", hi[:], 1.0)
    pm = k.ps([128, TW], F32, "pm")
    for it in range(30):
        k.tt("dve", mid[:], lo[:], hi[:], ALU.add)
        k.ts("dve", mid[:], mid[:], 0.5, ALU.mult)
        k.ts("dve", cmpb[:], aa[:], mid[:, 0:1], ALU.is_ge)
        k.reduce("dve", cnt[:], cmpb[:], ALU.add)
        k.mm(pm[:, 0:1], mb[:], cnt[:])
        k.ts("dve", cc[:], pm[:, 0:1], float(cap) - 0.5, ALU.is_ge)
        k.tt("dve", d1[:], hi[:], mid[:], ALU.subtract)
        k.stt("dve", hi[:], d1[:], cc[:, 0:1], mid[:], ALU.mult, ALU.add)
        k.tt("dve", d1[:], mid[:], lo[:], ALU.subtract)
        k.stt("dve", lo[:], d1[:], cc[:, 0:1], lo[:], ALU.mult, ALU.add)
    k.mm(pm[:NE, 0:1], sl[:], lo[:])
    thr = k.sb([NE, 1], F32); k.copy("act", thr[:], pm[:NE, 0:1])
    af = k.sb([NE, T], F32); k.dma("sp", af[:], affT[:, :])
    mg = k.sb([NE, T], F32)
    k.ts("dve", mg[:], af[:], thr[:, 0:1], ALU.is_ge)
    k.tt("dve", mg[:], mg[:], af[:], ALU.mult)

    ST = min(ST, T)
    acc = k.sb([128, KC, ST], F32, "acc"); u2 = k.sb([128, KC, ST], BF16, "u2")
    wib = [k.sb([128, KC, 1024], BF16, "wib%d" % i) for i in range(2)]
    wob = [k.sb([128, 4, D], BF16, "wob%d" % i) for i in range(2)]
    stg = [k.sb([128, KC, 128], F32, "stg%d" % i) for i in range(2)]
    actb = k.sb([128, 4, TW], BF16, "actb")
    sgt = [k.sb([128, TW], F32, "sgt%d" % i) for i in range(2)]
    put = [k.sb([128, TW], F32, "put%d" % i) for i in range(2)]
    ysb = [k.sb([128, TW], F32, "ysb%d" % i) for i in range(2)]
    mgbs = k.sb([128, TW], F32, "mgbs")
    hb = k.sb([128, KC, TW], F32, "hb")
    pgt = [k.ps([128, TW], F32, "pgt%d" % i) for i in range(2)]
    ppu = [k.ps([128, TW], F32, "ppu%d" % i) for i in range(2)]
    py = [k.ps([128, TW], F32, "py%d" % i) for i in range(2)]
    u2_v = u2T.t.rearrange("(c p) t -> p c t", p=128)
    hm_v = hmidT.t.rearrange("(c p) t -> p c t", p=128)
    ho_v = houtT.t.rearrange("(c p) t -> p c t", p=128)
    rstd = k.sb([128, TW], F32)
    n = 0
    units = [(e, hf) for e in range(nexp) for hf in range(2)]
    for s0 in range(0, T, ST):
        sw = min(ST, T - s0)
        k.dma("sp", u2[:, :, 0:sw], u2T.v(u2_v[:, :, s0:s0 + sw]))
        for ui, (e, hf) in enumerate(units):
            wbi = wib[ui % 2]; wbo = wob[ui % 2]
            wi_v = win.t[e].rearrange("(c p) f -> p c f", p=128)
            wo_v = wout.t[e][hf * 512:(hf + 1) * 512, :].rearrange("(c p) f -> p c f", p=128)
            for part in range(2):
                for c0 in range(0, 512, 128):
                    s_ = stg[n % 2]; q_ = "pool" if n % 2 == 0 else "sp"; n += 1
                    src0 = part * 1024 + hf * 512 + c0
                    k.dma(q_, s_[:], win.v(wi_v[:, :, src0:src0 + 128]))
                    k.copy("dve", wbi[:, :, part * 512 + c0: part * 512 + c0 + 128], s_[:])
            for c0 in range(0, D, 256):
                s_ = stg[n % 2]; q_ = "pool" if n % 2 == 0 else "sp"; n += 1
                k.dma(q_, s_[:, 0:4, :], wout.v(wo_v[:, :, c0:c0 + 128]))
                k.dma(q_, s_[:, 4:8, :], wout.v(wo_v[:, :, c0 + 128:c0 + 256]))
                k.copy("dve", wbo[:, :, c0:c0 + 128], s_[:, 0:4, :])
                k.copy("dve", wbo[:, :, c0 + 128:c0 + 256], s_[:, 4:8, :])
            for t0 in range(0, sw, TW):
                tl = slice(t0, t0 + TW)
                k.mm(pm[:], slb[:, e, :], mg[:, s0 + t0: s0 + t0 + TW])
                k.copy("act", mgbs[:], pm[:])
                for hc in range(4):
                    b = hc % 2
                    for c in range(KC):
                        k.mm(pgt[b][:], wbi[:, c, hc * 128:(hc + 1) * 128], u2[:, c, tl], start=(c == 0), stop=(c == KC - 1))
                    for c in range(KC):
                        k.mm(ppu[b][:], wbi[:, c, 512 + hc * 128:512 + (hc + 1) * 128], u2[:, c, tl], start=(c == 0), stop=(c == KC - 1))
                    k.act(sgt[b][:], pgt[b][:], AF.Silu)
                    k.copy("act", put[b][:], ppu[b][:])
                    k.tt("dve", actb[:, hc, :], sgt[b][:], put[b][:], ALU.mult)
                for dc in range(KC):
                    b = dc % 2
                    for c in range(4):
                        k.mm(py[b][:], wbo[:, c, dc * 128:(dc + 1) * 128], actb[:, c, :], start=(c == 0), stop=(c == 3))
                    k.copy("act", ysb[b][:], py[b][:])
                    if ui == 0:
                        k.tt("dve", acc[:, dc, tl], ysb[b][:], mgbs[:], ALU.mult)
                    else:
                        k.tt("dve", ysb[b][:], ysb[b][:], mgbs[:], ALU.mult)
                        k.tt("dve", acc[:, dc, tl], acc[:, dc, tl], ysb[b][:], ALU.add)
        for t0 in range(0, sw, TW):
            tl = slice(t0, t0 + TW); gl = slice(s0 + t0, s0 + t0 + TW)
            k.dma("sp", hb[:], hmidT.v(hm_v[:, :, gl]))
            for dc in range(KC):
                k.stt("dve", hb[:, dc, :], acc[:, dc, tl], modt[:, 40 + dc:41 + dc], hb[:, dc, :], ALU.mult, ALU.add)
            if final:
                sqb = acc
                k.act(sqb[:, :, tl], hb[:], AF.Square)
                for c in range(KC):
                    k.mm(pm[:], ones[:], sqb[:, c, tl], start=(c == 0), stop=(c == KC - 1))
                k.ts("dve", rstd[:], pm[:], 1.0 / D, ALU.mult, EPS, ALU.add)
                k.act(rstd[:], rstd[:], AF.Sqrt)
                k.recip(rstd[:], rstd[:])
                for c in range(KC):
                    k.stt("dve", hb[:, c, :], hb[:, c, :], gfs[:, c:c + 1], rstd[:], ALU.mult, ALU.mult)
            k.dma("sp", houtT.v(ho_v[:, :, gl]), hb[:])
    return k.finish()


def D_consts():
    mblk = np.zeros((128, 128), np.float32)
    for p in range(128):
        mblk[p, (p // 8) * 8:(p // 8) * 8 + 8] = 1.0
    sel = np.zeros((128, NE), np.float32)
    for e in range(NE):
        sel[8 * e, e] = 1.0
    selb = np.zeros((NE, NE, 128), np.float32)
    for e in range(NE):
        selb[e, e, :] = 1.0
    return dict(mblk=mblk, sel=sel, selb=selb.reshape(NE, NE * 128))


def aff_layout(aff):
    nseq = aff.shape[0]
    return np.ascontiguousarray(aff.T.reshape(NE, 8, nseq // 8).reshape(128, nseq // 8))


def lay(v):
    v = np.asarray(v, np.float32).reshape(-1, 128)
    return np.ascontiguousarray(v.T)


SEQ = 16384
BATCH = 2
CTX = 256
NCORE = 8
TC = SEQ // 4
_PROG = {}


def _prog(key, fn):
    if key not in _PROG:
        _PROG[key] = fn()
    return _PROG[key]


def _run(nc, maps):
    res = run_bass_kernel_spmd(nc, maps, core_ids=list(range(NCORE)))
    return [{kk: np.asarray(v) for kk, v in r.items()} for r in res.results]


def _c(a, dt=np.float32):
    return np.ascontiguousarray(a, dtype=dt)


def _hyena(l, inputs, zsrc, n):
    p = {kk: np.asarray(inputs[kk][l], np.float32) for kk in
         ("hyena_short_w", "hyena_short_b", "filt_w1", "filt_b1", "filt_freq", "filt_w2", "filt_b2", "filt_w3", "hyena_skip")}
    mapsS, mapsF = [], []
    for i in range(NCORE):
        ch0 = CPC * i
        rows = np.concatenate([np.arange(ch0, ch0 + CPC) + s * 256 for s in range(3)])
        zr = np.stack([zsrc[b][rows] for b in range(BATCH)], 1)
        zpad = np.pad(zr.reshape(96 * 2, n), ((0, 0), (1, 1)))
        wsb = np.repeat(np.concatenate([p["hyena_short_w"][:, rows].T, p["hyena_short_b"][rows][:, None]], 1), 2, axis=0)
        mapsS.append({"zp": _c(zpad), "wsb": _c(wsb)})
        zT, dd = hyena_consts(n, ch0)
        cols = lambda d: np.concatenate([o * 512 + d * 256 + np.arange(ch0, ch0 + CPC) for o in range(2)])
        w3 = np.stack([p["filt_w3"][:, cols(0)], p["filt_w3"][:, cols(1)]])
        pv = np.stack([p["filt_b1"], p["filt_freq"][0], p["filt_b2"], p["filt_freq"][1]], 1)
        mapsF.append({"zT": zT, "w1": _c(p["filt_w1"]), "w2": _c(p["filt_w2"]), "w3": _c(w3), "pv": _c(pv), "dec": dd})
    rS = _run(_prog(("HS", n), lambda: build_HS(192, n)), mapsS)
    rF = _run(_prog(("HF", n), lambda: build_HF(n)), mapsF)
    zs = [r["zo"].reshape(3, CPC, 2, n) for r in rS]
    cur = [z[2] for z in zs]
    for o in range(2):
        mapsC = []
        for i in range(NCORE):
            dsk = p["hyena_skip"][o, CPC * i:CPC * (i + 1)]
            mapsC.append({"G": np.ascontiguousarray(rF[i]["G"][o]), "Ur": to_blocks(cur[i], rev=True), "Vn": to_blocks(cur[i]),
                          "X": to_blocks(zs[i][o]), "Dbc": _c(np.broadcast_to(dsk[None, :], (128, CPC)))})
        rC = _run(_prog(("HC", n), lambda: build_HC(n)), mapsC)
        cur = [from_blocks(r["Y"]) for r in rC]
    return [np.concatenate([cur[i][:, b, :] for i in range(NCORE)], 0) for b in range(BATCH)]


def _attention(qd, kd, vd, mq, mk, mv, Tq, Tk):
    bf = qd[0].dtype
    maps = []
    for i in range(NCORE):
        b, h = divmod(i, 4)
        maps.append({"QT": np.ascontiguousarray(qd[b][h].reshape(2, 64, Tq)), "KT": np.ascontiguousarray(kd[b][h].reshape(2, 64, Tk)),
                     "V": np.ascontiguousarray(np.stack([vd[b][:, h * 128:(h + 1) * 128]] * 2))})
    r = _run(_prog(("Bd", Tq, Tk), lambda: build_attn(2, 64, 128, Tq, Tk, 0.125)), maps)
    od = [np.concatenate([r[b * 4 + h]["OT"] for h in range(4)], 0) for b in range(BATCH)]
    maps = []
    for i in range(NCORE):
        b, h = divmod(i, 4)
        maps.append({"QT": np.ascontiguousarray(mq[b][h][None]), "KT": np.ascontiguousarray(mk[b][h][None]),
                     "V": np.ascontiguousarray(mv[b][:, h * 64:(h + 1) * 64][None])})
    r = _run(_prog(("Bm", Tq, Tk), lambda: build_attn(1, 96, 64, Tq, Tk, 96 ** -0.5)), maps)
    om = [np.concatenate([r[b * 4 + h]["OT"][0] for h in range(4)], 0) for b in range(BATCH)]
    return od, om


def kernel(**inputs):
    inp = {kk: np.asarray(v) for kk, v in inputs.items()}
    x = inp["x"].astype(np.float32)
    cv = np.stack([lay(inp["c"][0]), lay(inp["c"][1]), lay(inp["c_ctx"])], -1).reshape(128, KC * 3)
    r0 = _run(_prog("P0", build_P0), [{"cvec": _c(cv), "ada_w": _c(inp["ada_w"]), "ada_b": np.stack([lay(inp["ada_b"][l]) for l in range(L)])}] * NCORE)
    mods = r0[0]["mods"].reshape(L, 128, 48, 3)
    h_lat = [x[b] for b in range(BATCH)]
    h_ctx = [inp["ctx"][b].astype(np.float32) for b in range(BATCH)]
    dconst = D_consts()
    out = np.zeros((BATCH, SEQ, 1024), np.float32)
    for l in range(L):
        last = l == L - 1
        lam_init = 0.8 - 0.6 * math.exp(-0.3 * l)
        w_in = inp["w_in"][l].astype(np.float32)
        aw = A_weights(w_in, inp["mla_w_uq"][l], inp["mla_w_ukv"][l], inp["mla_q_norm_g"][l], inp["mla_kv_norm_g"][l], inp["norm_mix_g"][l])
        mapsL, mapsX = [], []
        rc_ctx = rope_consts(np.arange(CTX), use_rope=False)
        for i in range(NCORE):
            b, j = divmod(i, 4)
            m = {"hT": _c(h_lat[b][j * TC:(j + 1) * TC].T), "mods": _c(mods[l, :, :, b])}
            m.update(rope_consts(np.arange(j * TC, (j + 1) * TC))); m.update(aw)
            mapsL.append(m)
            m = {"hT": _c(h_ctx[b].T), "mods": _c(mods[l, :, :, 2])}
            m.update(rc_ctx); m.update(aw)
            mapsX.append(m)
        rA = _run(_prog(("A", TC), lambda: build_A(TC)), mapsL)
        rX = _run(_prog(("A", CTX), lambda: build_A(CTX)), mapsX)
        cat = lambda name, b, ax: np.concatenate([rA[b * 4 + j][name] for j in range(4)], ax)
        qk_l = [cat("qkT", b, 2) for b in range(BATCH)]
        qk_c = [rX[b * 4]["qkT"] for b in range(BATCH)]
        v_all = [np.concatenate([rX[b * 4]["vtok"], cat("vtok", b, 0)], 0) for b in range(BATCH)]
        mq_l = [cat("mqT", b, 2) for b in range(BATCH)]
        mk_all = [np.concatenate([rX[b * 4]["mkT"], cat("mkT", b, 2)], 2) for b in range(BATCH)]
        mv_all = [np.concatenate([rX[b * 4]["mvtok"], cat("mvtok", b, 0)], 0) for b in range(BATCH)]
        kd_all = [np.concatenate([qk_c[b][4:8], qk_l[b][4:8]], 2) for b in range(BATCH)]
        od_l, om_l = _attention([qk_l[b][0:4] for b in range(BATCH)], kd_all, v_all, mq_l, mk_all, mv_all, SEQ, SEQ + CTX)
        yh_l = _hyena(l, inp, [cat("hyT", b, 1) for b in range(BATCH)], SEQ)
        glu_l = [np.pad(cat("gluT", b, 1), ((0, 0), (15, 15))) for b in range(BATCH)]
        if not last:
            od_c, om_c = _attention([qk_c[b][0:4] for b in range(BATCH)], [qk_c[b][4:8] for b in range(BATCH)],
                                    [rX[b * 4]["vtok"] for b in range(BATCH)], [rX[b * 4]["mqT"] for b in range(BATCH)],
                                    [rX[b * 4]["mkT"] for b in range(BATCH)], [rX[b * 4]["mvtok"] for b in range(BATCH)], CTX, CTX)
            yh_c = _hyena(l, inp, [rX[b * 4]["hyT"] for b in range(BATCH)], CTX)
            glu_c = [np.pad(rX[b * 4]["gluT"], ((0, 0), (15, 15))) for b in range(BATCH)]
        cwl = inp["conf_dw_w"][l].astype(np.float32)
        lng, lnb = inp["conf_ln_g"][l], inp["conf_ln_b"][l]
        cshared = {"gmix": lay(inp["norm_mix_g"][l]), "gffn": lay(inp["norm_ffn_g"][l]), "dlam": _c(inp["diff_lambda"][l].reshape(1, 256)),
                   "gsub": _c(inp["diff_subln_g"][l].reshape(128, 1)),
                   "cw": _c(cwl.T.reshape(2, 128, 31).transpose(1, 0, 2).reshape(128, 62)),
                   "lnp": _c(np.stack([lng[:128], lng[128:], lnb[:128], lnb[128:]], 1)),
                   "wg": _c(w_in[:, 3232:7328]), "wbr": _c(inp["w_branch"][l]), "wo": _c(inp["w_out"][l]), "wr": _c(inp["w_router"][l])}
        maps = []
        for i in range(NCORE):
            b, j = divmod(i, 4)
            tk = slice(j * TC, (j + 1) * TC)
            m = {"hT": _c(h_lat[b][tk].T), "mods": _c(mods[l, :, :, b]), "odT": _c(od_l[b][:, :, tk]), "yhT": _c(yh_l[b][:, tk]),
                 "ymT": _c(om_l[b][:, tk]), "glup": _c(glu_l[b][:, j * TC: (j + 1) * TC + 30])}
            m.update(cshared); maps.append(m)
        rC = _run(_prog(("C", TC, l), lambda: build_C(TC, lam_init)), maps)
        dshared = {"win": _c(inp["w_exp_in"][l]), "wout": _c(inp["w_exp_out"][l]), "gfin": lay(inp["final_norm_g"])}
        dshared.update(dconst)
        aff_b = [np.concatenate([rC[b * 4 + j]["aff"] for j in range(4)], 0) for b in range(BATCH)]
        maps = []
        for i in range(NCORE):
            b, j = divmod(i, 4)
            m = {"hmidT": rC[i]["hmidT"], "u2T": rC[i]["u2T"], "affall": aff_layout(aff_b[b]), "affT": _c(aff_b[b][j * TC:(j + 1) * TC].T),
                 "mods": _c(mods[l, :, :, b])}
            m.update(dshared); maps.append(m)
        rD = _run(_prog(("D", TC, last), lambda: build_D(TC, SEQ, 2 * SEQ // NE, last, ST=1024)), maps)
        for i in range(NCORE):
            b, j = divmod(i, 4)
            res = rD[i]["houtT"].T
            if last:
                out[b, j * TC:(j + 1) * TC] = res
            else:
                h_lat[b] = h_lat[b].copy() if j == 0 else h_lat[b]
                h_lat[b][j * TC:(j + 1) * TC] = res
        if not last:
            maps = []
            for i in range(NCORE):
                b = i // 4
                m = {"hT": _c(h_ctx[b].T), "mods": _c(mods[l, :, :, 2]), "odT": _c(od_c[b]), "yhT": _c(yh_c[b]), "ymT": _c(om_c[b]), "glup": _c(glu_c[b])}
                m.update(cshared); maps.append(m)
            rCc = _run(_prog(("C", CTX, l), lambda: build_C(CTX, lam_init)), maps)
            maps = []
            for i in range(NCORE):
                b = i // 4
                a_ = rCc[b * 4]["aff"]
                m = {"hmidT": rCc[i]["hmidT"], "u2T": rCc[i]["u2T"], "affall": aff_layout(a_), "affT": _c(a_.T), "mods": _c(mods[l, :, :, 2])}
                m.update(dshared); maps.append(m)
            rDc = _run(_prog(("D", CTX, False), lambda: build_D(CTX, CTX, 2 * CTX // NE, False, ST=1024)), maps)
            h_ctx = [rDc[b * 4]["houtT"].T.copy() for b in range(BATCH)]
    return out
```

```python
import math
import os
import numpy as np
import concourse.bass as bass
import concourse.mybir as mybir
from concourse.bass_utils import run_bass_kernel_spmd

F32 = mybir.dt.float32
BF16 = mybir.dt.bfloat16
I32 = mybir.dt.int32
AF = mybir.ActivationFunctionType
ALU = mybir.AluOpType
AX = mybir.AxisListType

COMPUTE = ("pe", "act", "dve", "pool")


class Buf:
    __slots__ = ("name", "last_w", "readers", "sem_in", "sem_out")

    def __init__(self, name):
        self.name = name
        self.last_w = None
        self.readers = []
        self.sem_in = None
        self.sem_out = None


class Op:
    __slots__ = ("eng", "fn", "deps", "signals", "sem", "count", "is_dma", "ndma", "idx")


class Sched:
    def __init__(self, nc):
        self.nc = nc
        self.ops = []
        self.eng_sem = {}
        self.dma_sems = []
        self.out_ops = []

    def _record(self, eng, fn, reads, writes, is_dma=False, ndma=1, sem_owner=None, acc=False):
        op = Op()
        op.eng = eng
        op.fn = fn
        op.is_dma = is_dma
        op.ndma = ndma
        op.signals = is_dma
        op.sem = None
        op.count = 0
        op.idx = len(self.ops)
        reads = list({id(b): b for b in reads}.values())
        writes = list({id(b): b for b in writes}.values())
        deps = set()
        for b in reads:
            if b.last_w is not None:
                deps.add(b.last_w)
        for b in writes:
            if b.last_w is not None:
                deps.add(b.last_w)
            for r in b.readers:
                deps.add(r)
        deps.discard(op.idx)
        if eng == "pe":
            deps = {d for d in deps if self.ops[d].eng != "pe" or self.ops[d].is_dma}
        op.deps = deps
        for b in writes:
            b.last_w = op.idx
            b.readers = []
        for b in reads:
            if b.last_w != op.idx:
                if not is_dma:
                    b.readers = [r for r in b.readers if self.ops[r].is_dma or self.ops[r].eng != eng]
                b.readers.append(op.idx)
        if is_dma:
            op.sem = sem_owner
        self.ops.append(op)
        return op

    def op(self, eng, fn, reads=(), writes=()):
        return self._record(eng, fn, list(reads), list(writes))

    def dma(self, queue, pairs, reads=(), writes=(), owner=None, kind="in"):
        assert owner is not None
        key = (owner, kind)
        op = self._record(queue, pairs, list(reads), list(writes), is_dma=True,
                          ndma=len(pairs), sem_owner=key)
        return op

    def emit(self):
        nc = self.nc
        ops = self.ops
        for o in ops:
            for d in o.deps:
                ops[d].signals = True
        last_of = {}
        for o in ops:
            last_of[o.eng] = o.idx
        final_deps = set(last_of.values())
        for o in ops:
            if o.is_dma:
                final_deps.add(o.idx)
        for d in final_deps:
            ops[d].signals = True
        sem_objs = {}

        def get_sem(key):
            if key not in sem_objs:
                sem_objs[key] = nc.alloc_semaphore(name="s_%d" % len(sem_objs))
            return sem_objs[key]

        counts = {}
        for o in ops:
            if o.is_dma:
                key = ("dma", id(o.sem[0]), o.sem[1])
                o.sem = key
                counts[key] = counts.get(key, 0) + 16 * o.ndma
                o.count = counts[key]
            elif o.signals:
                key = ("eng", o.eng)
                o.sem = key
                counts[key] = counts.get(key, 0) + 1
                o.count = counts[key]
        self.n_sems = len(set(o.sem for o in ops if o.sem is not None))
        by_eng = {}
        for o in ops:
            by_eng.setdefault(o.eng, []).append(o)
        handles = {"pe": nc.tensor, "act": nc.scalar, "dve": nc.vector, "pool": nc.gpsimd, "sp": nc.sync}
        final_waits = {}
        for d in final_deps:
            od = ops[d]
            final_waits[od.sem] = max(final_waits.get(od.sem, 0), od.count)
        self.n_waits = 0

        def run_engine(ename, h):
            known = {}
            for o in by_eng.get(ename, []):
                need = {}
                for d in o.deps:
                    od = ops[d]
                    need[od.sem] = max(need.get(od.sem, 0), od.count)
                for key, v in need.items():
                    if known.get(key, 0) >= v:
                        continue
                    h.wait_ge(get_sem(key), v)
                    known[key] = v
                    self.n_waits += 1
                if o.is_dma:
                    s = get_sem(o.sem)
                    for (oap, iap) in o.fn:
                        h.dma_start(out=oap, in_=iap).then_inc(s, 16)
                else:
                    ins = o.fn(h)
                    if o.signals:
                        ins.then_inc(get_sem(o.sem), 1)
            if ename == "sp":
                for key, v in final_waits.items():
                    if known.get(key, 0) >= v:
                        continue
                    h.wait_ge(get_sem(key), v)

        with nc.Block() as block:
            @block.sync
            def _(e):
                run_engine("sp", e)

            @block.tensor
            def _(e):
                run_engine("pe", e)

            @block.scalar
            def _(e):
                run_engine("act", e)

            @block.vector
            def _(e):
                run_engine("dve", e)

            @block.gpsimd
            def _(e):
                run_engine("pool", e)


import contextlib


class View:
    __slots__ = ("buf", "ap")

    def __init__(self, buf, ap):
        self.buf = buf
        self.ap = ap


class Tile:
    def __init__(self, t, name):
        self.t = t
        self.buf = Buf(name)

    def __getitem__(self, key):
        return View(self.buf, self.t[key])

    def v(self, ap):
        return View(self.buf, ap)


def _ap(x):
    return x.ap if isinstance(x, View) else x


def _bufs(*xs):
    return [x.buf for x in xs if isinstance(x, View)]


class KB:
    def __init__(self):
        self.nc = bass.Bass("TRN2", target_bir_lowering=False)
        self.S = Sched(self.nc)
        self.st = contextlib.ExitStack()
        self.n = 0

    def sb(self, shape, dt, name=None):
        self.n += 1
        name = name or "sb%d" % self.n
        return Tile(self.st.enter_context(self.nc.sbuf_tensor(name, list(shape), dt)), name)

    def ps(self, shape, dt, name=None):
        self.n += 1
        name = name or "ps%d" % self.n
        return Tile(self.st.enter_context(self.nc.psum_tensor(name, list(shape), dt)), name)

    def dram(self, name, shape, dt, kind):
        return Tile(self.nc.dram_tensor(name, list(shape), dt, kind=kind), "d_" + name)

    def dma(self, q, out, in_):
        sbuf_side = in_ if out.buf.name.startswith("d_") else out
        kind = "in" if sbuf_side is out else "out"
        self.S.dma(q, [(out.ap, in_.ap)], reads=[in_.buf], writes=[out.buf], owner=sbuf_side.buf, kind=kind)

    def mm(self, out, lhsT, rhs, start=True, stop=True):
        o, l, r = out.ap, lhsT.ap, rhs.ap
        self.S.op("pe", lambda e: e.matmul(o, lhsT=l, rhs=r, start=start, stop=stop),
                  reads=[lhsT.buf, rhs.buf], writes=[out.buf])

    def transpose(self, out, in_, ident):
        o, i, d = out.ap, in_.ap, ident.ap
        self.S.op("pe", lambda e: e.transpose(o, i, d), reads=[in_.buf, ident.buf], writes=[out.buf])

    def act(self, out, in_, func, bias=None, scale=None, accum=None):
        kw = {}
        if bias is not None:
            kw["bias"] = _ap(bias)
        if scale is not None:
            kw["scale"] = _ap(scale)
        if accum is not None:
            kw["accum_out"] = accum.ap
        o, i = out.ap, in_.ap
        self.S.op("act", lambda e: e.activation(out=o, in_=i, func=func, **kw),
                  reads=[in_.buf] + _bufs(bias, scale), writes=[out.buf] + _bufs(accum))

    def tt(self, eng, out, in0, in1, op):
        o, a, b = out.ap, in0.ap, in1.ap
        self.S.op(eng, lambda e: e.tensor_tensor(out=o, in0=a, in1=b, op=op),
                  reads=[in0.buf, in1.buf], writes=[out.buf])

    def ts(self, eng, out, in0, s1, op0, s2=None, op1=None, accum=None):
        o, a = out.ap, in0.ap
        kw = {}
        if op1 is not None:
            kw["op1"] = op1
        if accum is not None:
            kw["accum_out"] = accum.ap
        s1a, s2a = _ap(s1), _ap(s2)
        self.S.op(eng, lambda e: e.tensor_scalar(out=o, in0=a, scalar1=s1a, scalar2=s2a, op0=op0, **kw),
                  reads=[in0.buf] + _bufs(s1, s2), writes=[out.buf] + _bufs(accum))

    def stt(self, eng, out, in0, scalar, in1, op0, op1):
        o, a, b = out.ap, in0.ap, in1.ap
        sa = _ap(scalar)
        self.S.op(eng, lambda e: e.scalar_tensor_tensor(out=o, in0=a, scalar=sa, in1=b, op0=op0, op1=op1),
                  reads=[in0.buf, in1.buf] + _bufs(scalar), writes=[out.buf])

    def copy(self, eng, out, in_):
        o, i = out.ap, in_.ap
        if eng == "act":
            self.S.op("act", lambda e: e.copy(out=o, in_=i), reads=[in_.buf], writes=[out.buf])
        else:
            self.S.op(eng, lambda e: e.tensor_copy(out=o, in_=i), reads=[in_.buf], writes=[out.buf])

    def recip(self, out, in_):
        o, i = out.ap, in_.ap
        self.S.op("dve", lambda e: e.reciprocal(out=o, in_=i), reads=[in_.buf], writes=[out.buf])

    def memset(self, eng, out, val):
        o = out.ap
        self.S.op(eng, lambda e: e.memset(o, val), writes=[out.buf])

    def reduce(self, eng, out, in_, op, axis=None):
        o, i = out.ap, in_.ap
        ax = axis or AX.X
        self.S.op(eng, lambda e: e.tensor_reduce(out=o, in_=i, axis=ax, op=op), reads=[in_.buf], writes=[out.buf])

    def finish(self):
        self.S.emit()
        self.st.close()
        return self.nc


D = 1024
KC = 8
L = 2


def build_P0():
    k = KB()
    cvec = k.dram("cvec", [128, KC * 3], F32, "ExternalInput")
    ada_w = k.dram("ada_w", [L, D, 6 * D], F32, "ExternalInput")
    ada_b = k.dram("ada_b", [L, 128, 48], F32, "ExternalInput")
    mods = k.dram("mods", [L, 128, 48 * 3], F32, "ExternalOutput")
    cv = k.sb([128, KC, 3], F32)
    k.dma("sp", cv[:], cvec.v(cvec.t.rearrange("p (c j) -> p c j", j=3)))
    s = k.sb([128, KC, 3], F32)
    k.act(s[:], cv[:], AF.Silu)
    wst = [k.sb([128, KC, 512], F32, "wst%d" % i) for i in range(2)]
    ps = [k.ps([128, 512], F32, "ps%d" % i) for i in range(2)]
    it = 0
    for l in range(L):
        bt = k.sb([128, 48], F32, "bt%d" % l)
        k.dma("sp", bt[:], ada_b[l, :, :])
        mo = k.sb([128, 48, 3], F32, "mo%d" % l)
        wv = ada_w.t[l].rearrange("(c p) f -> p c f", p=128)
        for cg in range(12):
            w = wst[cg % 2]
            k.dma("sp", w[:], ada_w.v(wv[:, :, cg * 512:(cg + 1) * 512]))
            for f in range(4):
                fi = cg * 4 + f
                p = ps[it % 2]; it += 1
                for c in range(KC):
                    k.mm(p[:, 0:3], w[:, c, f * 128:(f + 1) * 128], s[:, c, :], start=(c == 0), stop=(c == KC - 1))
                k.ts("dve", mo[:, fi, :], p[:, 0:3], bt[:, fi:fi + 1], ALU.add)
        k.dma("sp", mods.v(mods.t[l].rearrange("p (f j) -> p f j", j=3)), mo[:])
    return k.finish()


def lay(v):
    v = np.asarray(v, np.float32).reshape(-1, 128)
    return np.ascontiguousarray(v.T)


D = 1024
KC = 8
EPS = 1e-6
NA = 3072


def build_A(T):
    k = KB()
    TW = min(512, T)
    NT = T // TW
    hT = k.dram("hT", [D, T], F32, "ExternalInput")
    mods = k.dram("mods", [128, 48], F32, "ExternalInput")
    gmix = k.dram("gmix", [128, KC], F32, "ExternalInput")
    wA = k.dram("wA", [D, NA], F32, "ExternalInput")
    wv = k.dram("wv", [D, 512], F32, "ExternalInput")
    cosd = k.dram("cosd", [128, T], F32, "ExternalInput"); sind = k.dram("sind", [128, T], F32, "ExternalInput")
    cosq = k.dram("cosq", [96, T], F32, "ExternalInput"); sinq = k.dram("sinq", [96, T], F32, "ExternalInput")
    perm = k.dram("perm", [128, 128], F32, "ExternalInput")
    permq = k.dram("permq", [96, 96], F32, "ExternalInput")
    perm32 = k.dram("perm32", [32, 32], F32, "ExternalInput")
    gq = k.dram("gq", [128, 2], F32, "ExternalInput")
    gkv = k.dram("gkv", [128, 1], F32, "ExternalInput")
    wuq = k.dram("wuq", [256, 384], F32, "ExternalInput")
    wuk = k.dram("wuk", [128, 256], F32, "ExternalInput")
    wuv = k.dram("wuv", [128, 256], F32, "ExternalInput")
    qkT = k.dram("qkT", [8, 128, T], BF16, "ExternalOutput")
    vtok = k.dram("vtok", [T, 512], BF16, "ExternalOutput")
    hyT = k.dram("hyT", [768, T], F32, "ExternalOutput")
    gluT = k.dram("gluT", [256, T], F32, "ExternalOutput")
    mqT = k.dram("mqT", [4, 96, T], BF16, "ExternalOutput")
    mkT = k.dram("mkT", [4, 96, T], BF16, "ExternalOutput")
    mvtok = k.dram("mvtok", [T, 256], BF16, "ExternalOutput")

    ones = k.sb([128, 128], F32); k.memset("dve", ones[:], 1.0)
    modt = k.sb([128, 48], F32); k.dma("sp", modt[:], mods[:, :])
    gm = k.sb([128, KC], F32); k.dma("sp", gm[:], gmix[:, :])
    Acoef = k.sb([128, KC], F32)
    k.ts("dve", Acoef[:], modt[:, 8:16], 1.0, ALU.add)
    k.tt("dve", Acoef[:], Acoef[:], gm[:], ALU.mult)

    def load_bf(dr, shape, view=None):
        f = k.sb(shape, F32); b = k.sb(shape, BF16)
        k.dma("pool", f[:], dr[:, :] if view is None else dr.v(view))
        k.copy("dve", b[:], f[:])
        return b
    permb = load_bf(perm, [128, 128]); permqb = load_bf(permq, [96, 96])
    perm32b = load_bf(perm32, [32, 32])
    wuqb = load_bf(wuq, [128, 2, 384], wuq.t.rearrange("(j p) f -> p j f", p=128))
    wukb = load_bf(wuk, [128, 256]); wuvb = load_bf(wuv, [128, 256])
    gqs = k.sb([128, 2], F32); k.dma("sp", gqs[:], gq[:, :])
    gkvs = k.sb([128, 1], F32); k.dma("sp", gkvs[:], gkv[:, :])

    uT = k.sb([128, KC, T], BF16, "uT")
    hT_v = hT.t.rearrange("(c p) t -> p c t", p=128)
    ps_ssq = k.ps([128, TW], F32)
    ht = k.sb([128, KC, TW], F32, "ht"); sq = k.sb([128, KC, TW], F32, "sq")
    rstd = k.sb([128, TW], F32, "rstd"); tmp = k.sb([128, TW], F32, "tmpn")

    def rstd_from(ps, n_feat):
        k.ts("dve", rstd[:], ps[:], 1.0 / n_feat, ALU.mult, EPS, ALU.add)
        k.act(rstd[:], rstd[:], AF.Sqrt)
        k.recip(rstd[:], rstd[:])

    for ti in range(NT):
        ts_ = slice(ti * TW, (ti + 1) * TW)
        k.dma("sp", ht[:], hT.v(hT_v[:, :, ts_]))
        k.act(sq[:], ht[:], AF.Square)
        for c in range(KC):
            k.mm(ps_ssq[:], ones[:], sq[:, c, :], start=(c == 0), stop=(c == KC - 1))
        rstd_from(ps_ssq, D)
        for c in range(KC):
            k.tt("dve", tmp[:], ht[:, c, :], rstd[:], ALU.mult)
            k.ts("dve", uT[:, c, ts_], tmp[:], Acoef[:, c:c + 1], ALU.mult, modt[:, c:c + 1], ALU.add)

    wst = [k.sb([128, KC, 512], F32, "wst%d" % i) for i in range(2)]
    wbf = [k.sb([128, KC, 512], BF16, "wbf%d" % i) for i in range(2)]
    pz = [k.ps([128, TW], F32, "pz%d" % i) for i in range(3)]
    pr = [k.ps([128, TW], F32, "pr%d" % i) for i in range(2)]
    cst = [k.sb([128, TW], F32, "cs%d" % i) for i in range(2)]
    snt = [k.sb([128, TW], F32, "sn%d" % i) for i in range(2)]
    zb = [k.sb([128, TW], BF16, "zb%d" % i) for i in range(2)]
    t1 = [k.sb([128, TW], F32, "t1%d" % i) for i in range(2)]
    t2 = [k.sb([128, TW], F32, "t2%d" % i) for i in range(2)]
    ob = [k.sb([128, TW], BF16, "ob%d" % i) for i in range(2)]
    of = [k.sb([128, TW], F32, "of%d" % i) for i in range(2)]
    cqs = k.sb([128, 2, TW], F32, "cqs"); cqn = k.sb([128, 2, TW], BF16, "cqn")
    ckn = k.sb([128, TW], BF16, "ckn")
    st = {"i": 0, "z": 0}
    wA_v = wA.t.rearrange("(c p) f -> p c f", p=128)

    def proj(wb, f, ts_):
        p = pz[st["z"] % 3]; st["z"] += 1
        for c in range(KC):
            k.mm(p[:], wb[:, c, f * 128:(f + 1) * 128], uT[:, c, ts_], start=(c == 0), stop=(c == KC - 1))
        return p

    def rope(pview, R, cos_d, sin_d, pm, ts_, outs):
        b = st["i"] % 2; st["i"] += 1
        k.dma("sp", cst[b][:R, :], cos_d[:, ts_])
        k.dma("sp", snt[b][:R, :], sin_d[:, ts_])
        k.copy("act", t1[b][:R, :], pview)
        k.copy("dve", zb[b][:R, :], t1[b][:R, :])
        k.mm(pr[b][:R, :], pm[:R, :R], zb[b][:R, :])
        k.copy("act", t2[b][:R, :], pr[b][:R, :])
        k.tt("dve", t1[b][:R, :], t1[b][:R, :], cst[b][:R, :], ALU.mult)
        k.tt("dve", t2[b][:R, :], t2[b][:R, :], snt[b][:R, :], ALU.mult)
        k.tt("dve", ob[b][:R, :], t1[b][:R, :], t2[b][:R, :], ALU.add)
        for o in outs:
            k.dma("sp", o, ob[b][:R, :])

    def store_f32(pview, R, dst):
        b = st["i"] % 2; st["i"] += 1
        k.copy("act", of[b][:R, :], pview)
        k.dma("sp", dst, of[b][:R, :])

    for g in range(6):
        wb = wbf[g % 2]
        k.dma("pool", wst[g % 2][:], wA.v(wA_v[:, :, g * 512:(g + 1) * 512]))
        k.copy("dve", wb[:], wst[g % 2][:])
        for ti in range(NT):
            ts_ = slice(ti * TW, (ti + 1) * TW)
            if g < 2:
                for hd in range(4):
                    p = proj(wb, hd, ts_)
                    rope(p[:], 128, cosd, sind, permb, ts_, [qkT[g * 4 + hd, :, ts_]])
            elif g == 2:
                for f in range(4):
                    p = proj(wb, f, ts_)
                    store_f32(p[:], 128, hyT[f * 128:(f + 1) * 128, ts_])
            elif g == 3:
                for f in range(2):
                    p = proj(wb, f, ts_)
                    store_f32(p[:], 128, hyT[512 + f * 128:512 + (f + 1) * 128, ts_])
                for j in range(2):
                    p = proj(wb, 2 + j, ts_)
                    k.copy("act", cqs[:, j, :], p[:])
                k.act(sq[:, 0:2, :], cqs[:], AF.Square)
                for j in range(2):
                    k.mm(ps_ssq[:], ones[:], sq[:, j, :], start=(j == 0), stop=(j == 1))
                rstd_from(ps_ssq, 256)
                for j in range(2):
                    k.stt("dve", cqn[:, j, :], cqs[:, j, :], gqs[:, j:j + 1], rstd[:], ALU.mult, ALU.mult)
                for h in range(4):
                    p = pz[st["z"] % 3]; st["z"] += 1
                    for j in range(2):
                        k.mm(p[:96, :], wuqb[:, j, h * 96:(h + 1) * 96], cqn[:, j, :], start=(j == 0), stop=(j == 1))
                    rope(p[:96, :], 96, cosq, sinq, permqb, ts_, [mqT[h, :, ts_]])
            elif g == 4:
                for j in range(2):
                    pa = proj(wb, j, ts_)
                    pb = proj(wb, 2 + j, ts_)
                    b = st["i"] % 2; st["i"] += 1
                    k.act(t1[b][:], pb[:], AF.Sigmoid)
                    k.copy("act", t2[b][:], pa[:])
                    k.tt("dve", of[b][:], t1[b][:], t2[b][:], ALU.mult)
                    k.dma("sp", gluT[j * 128:(j + 1) * 128, ts_], of[b][:])
            else:
                p = proj(wb, 0, ts_)
                k.copy("act", cqs[:, 0, :], p[:])
                k.act(sq[:, 0, :], cqs[:, 0, :], AF.Square)
                k.mm(ps_ssq[:], ones[:], sq[:, 0, :])
                rstd_from(ps_ssq, 128)
                k.stt("dve", ckn[:], cqs[:, 0, :], gkvs[:, 0:1], rstd[:], ALU.mult, ALU.mult)
                pk = proj(wb, 1, ts_)
                b = st["i"] % 2; st["i"] += 1
                k.dma("sp", cst[b][:32, :], cosq[64:96, ts_])
                k.dma("sp", snt[b][:32, :], sinq[64:96, ts_])
                k.copy("act", t1[b][:32, :], pk[:32, :])
                k.copy("dve", zb[b][:32, :], t1[b][:32, :])
                k.mm(pr[b][:32, :], perm32b[:, :], zb[b][:32, :])
                k.copy("act", t2[b][:32, :], pr[b][:32, :])
                k.tt("dve", t1[b][:32, :], t1[b][:32, :], cst[b][:32, :], ALU.mult)
                k.tt("dve", t2[b][:32, :], t2[b][:32, :], snt[b][:32, :], ALU.mult)
                k.tt("dve", ob[b][:32, :], t1[b][:32, :], t2[b][:32, :], ALU.add)
                for h in range(4):
                    k.dma("sp", mkT[h, 64:96, ts_], ob[b][:32, :])
                for h in range(4):
                    p = pz[st["z"] % 3]; st["z"] += 1
                    k.mm(p[:64, :], wukb[:, h * 64:(h + 1) * 64], ckn[:])
                    b = st["i"] % 2; st["i"] += 1
                    k.copy("act", ob[b][:64, :], p[:64, :])
                    k.dma("sp", mkT[h, 0:64, ts_], ob[b][:64, :])
                for s in range(TW // 128):
                    p = pz[st["z"] % 3]; st["z"] += 1
                    k.mm(p[:, 0:256], ckn[:, s * 128:(s + 1) * 128], wuvb[:])
                    b = st["i"] % 2; st["i"] += 1
                    k.copy("act", ob[b][:, 0:256], p[:, 0:256])
                    k.dma("sp", mvtok[ti * TW + s * 128: ti * TW + (s + 1) * 128, :], ob[b][:, 0:256])
    k.dma("pool", wst[0][:], wv.v(wv.t.rearrange("(c p) f -> p c f", p=128)))
    k.copy("dve", wbf[0][:], wst[0][:])
    ov = [k.sb([128, 512], BF16, "ov%d" % i) for i in range(2)]
    pvv = [k.ps([128, 512], F32, "pvv%d" % i) for i in range(2)] if TW < 512 else pz
    for tb in range(T // 128):
        b = tb % 2
        for c in range(KC):
            k.mm(pvv[b][:], uT[:, c, tb * 128:(tb + 1) * 128], wbf[0][:, c, :], start=(c == 0), stop=(c == KC - 1))
        k.copy("act", ov[b][:], pvv[b][:])
        k.dma("sp", vtok[tb * 128:(tb + 1) * 128, :], ov[b][:])
    return k.finish()


GRID_W = 64


def lay(v):
    v = np.asarray(v, np.float32).reshape(-1, 128)
    return np.ascontiguousarray(v.T)


def rope_consts(pos, use_rope=True):
    T = len(pos)
    row = (pos // GRID_W).astype(np.float32); col = (pos % GRID_W).astype(np.float32)
    def ang(nf):
        inv = (10000.0 ** (-np.arange(nf, dtype=np.float32) / nf)).astype(np.float32)
        return np.concatenate([row[:, None] * inv, col[:, None] * inv], -1)
    a16, a8 = ang(16), ang(8)
    cosd = np.ones((128, T), np.float32); sind = np.zeros((128, T), np.float32)
    cosq = np.ones((96, T), np.float32); sinq = np.zeros((96, T), np.float32)
    if use_rope:
        for r in range(128):
            j = r % 32
            cosd[r] = np.cos(a16[:, j]); sind[r] = np.sin(a16[:, j]) * (-1.0 if (r % 64) < 32 else 1.0)
        for r in range(32):
            j = r % 16
            cosq[64 + r] = np.cos(a8[:, j]); sinq[64 + r] = np.sin(a8[:, j]) * (-1.0 if r < 16 else 1.0)
    perm = np.zeros((128, 128), np.float32)
    for m in range(128):
        perm[m + 32 if (m % 64) < 32 else m - 32, m] = 1.0
    permq = np.zeros((96, 96), np.float32)
    for m in range(96):
        if m < 64:
            permq[m, m] = 1.0
        else:
            r = m - 64
            permq[64 + (r + 16 if r < 16 else r - 16), m] = 1.0
    perm32 = np.zeros((32, 32), np.float32)
    for r in range(32):
        perm32[r + 16 if r < 16 else r - 16, r] = 1.0
    return dict(cosd=cosd, sind=sind, cosq=cosq, sinq=sinq, perm=perm, permq=permq, perm32=perm32)


def A_weights(w_in, wuq, wukv, gq, gkv, gmix):
    dq, dk, dv = w_in[:, 0:512], w_in[:, 512:1024], w_in[:, 1024:1536]
    hy = w_in[:, 1536:2304]; cf = w_in[:, 2304:2816]; cq = w_in[:, 2816:3072]; ckv = w_in[:, 3072:3200]; kr = w_in[:, 3200:3232]
    pad = np.zeros((1024, 512 - 160), np.float32)
    wA = np.concatenate([dq, dk, hy[:, :512], hy[:, 512:], cq, cf, ckv, kr, pad], 1)
    kcols = np.concatenate([np.arange(h * 128, h * 128 + 64) for h in range(4)])
    vcols = kcols + 64
    return dict(wA=np.ascontiguousarray(wA, np.float32), wv=np.ascontiguousarray(dv, np.float32), wuq=np.ascontiguousarray(wuq, np.float32),
                wuk=np.ascontiguousarray(wukv[:, kcols], np.float32), wuv=np.ascontiguousarray(wukv[:, vcols], np.float32),
                gq=lay(gq), gkv=lay(gkv), gmix=lay(gmix))


def build_attn(nmap, dqk, dv, Tq, Tk, scale, LA=2):
    k = KB()
    QT = k.dram("QT", [nmap, dqk, Tq], BF16, "ExternalInput")
    KT = k.dram("KT", [nmap, dqk, Tk], BF16, "ExternalInput")
    V = k.dram("V", [nmap, Tk, dv], BF16, "ExternalInput")
    OT = k.dram("OT", [nmap, dv, Tq], F32, "ExternalOutput")
    NK = Tk // 128
    QW = min(512, Tq)
    NQ = Tq // QW
    NS = LA + 1
    onesf = k.sb([128, 128], F32); k.memset("dve", onesf[:], 1.0)
    ones = k.sb([128, 128], BF16); k.copy("dve", ones[:], onesf[:])
    kts = [k.sb([dqk, Tk], BF16, "kt%d" % i) for i in range(2)]
    vs = [k.sb([128, NK, dv], BF16, "v%d" % i) for i in range(2)]
    qs = [k.sb([dqk, QW], BF16, "q%d" % i) for i in range(2)]
    S = [k.ps([128, QW], F32, "S%d" % i) for i in range(NS)]
    P = [k.sb([128, QW], BF16, "P%d" % i) for i in range(NS)]
    O = [k.ps([dv, QW], F32, "O%d" % i) for i in range(2)]
    Sm = [k.ps([dv, QW], F32, "Sm%d" % i) for i in range(2)]
    osb = [k.sb([dv, QW], F32, "osb%d" % i) for i in range(2)]
    ssb = [k.sb([dv, QW], F32, "ssb%d" % i) for i in range(2)]
    accP = [k.sb([128, QW], F32, "accP%d" % i) for i in range(2)]
    steps = [(m, qb, t) for m in range(nmap) for qb in range(NQ) for t in range(NK)]

    def issue_qk(i):
        m, qb, t = steps[i]
        blk = m * NQ + qb
        if t == 0:
            if qb == 0:
                k.dma("sp", kts[m % 2][:], KT[m, :, :])
                vv_ = V.t[m].rearrange("(t p) d -> p t d", p=128)
                for t0 in range(0, NK, 64):
                    t1_ = min(NK, t0 + 64)
                    k.dma("pool", vs[m % 2][:, t0:t1_, :], V.v(vv_[:, t0:t1_, :]))
            k.dma("sp", qs[blk % 2][:], QT[m, :, qb * QW:(qb + 1) * QW])
        k.mm(S[i % NS][:], kts[m % 2][:, t * 128:(t + 1) * 128], qs[blk % 2][:])

    for i in range(min(LA, len(steps))):
        issue_qk(i)
    for i, (m, qb, t) in enumerate(steps):
        if i + LA < len(steps):
            issue_qk(i + LA)
        blk = m * NQ + qb
        b = i % NS
        o_ = O[blk % 2]; sm_ = Sm[blk % 2]
        k.act(P[b][:], S[b][:], AF.Exp, scale=scale)
        k.mm(o_[:], vs[m % 2][:, t, :], P[b][:], start=(t == 0), stop=(t == NK - 1))
        if t == 0:
            k.copy("dve", accP[blk % 2][:], P[b][:])
        else:
            k.tt("dve", accP[blk % 2][:], accP[blk % 2][:], P[b][:], ALU.add)
        if t == NK - 1:
            k.mm(sm_[:], onesf[:, :dv], accP[blk % 2][:])
            ob_ = osb[blk % 2]; sb_ = ssb[blk % 2]
            k.copy("act", sb_[:], sm_[:])
            k.recip(sb_[:], sb_[:])
            k.copy("act", ob_[:], o_[:])
            k.tt("dve", ob_[:], ob_[:], sb_[:], ALU.mult)
            k.dma("sp", OT[m, :, qb * QW:(qb + 1) * QW], ob_[:])
    return k.finish()


import math

CPC = 32


def build_HS(R, n, CH=2048):
    k = KB()
    zp = k.dram("zp", [R, n + 2], F32, "ExternalInput")
    wsb = k.dram("wsb", [R, 4], F32, "ExternalInput")
    zo = k.dram("zo", [R, n], F32, "ExternalOutput")
    CH = min(CH, n)
    r0 = 0
    i = 0
    while r0 < R:
        rr = min(128, R - r0)
        w = k.sb([rr, 4], F32, "w%d" % r0)
        k.dma("sp", w[:], wsb[r0:r0 + rr, :])
        for c0 in range(0, n, CH):
            zt = k.sb([128, CH + 2], F32, "zt%d" % (i % 2)) if i < 2 else zts[i % 2]
            ot = k.sb([128, CH], F32, "ot%d" % (i % 2)) if i < 2 else ots[i % 2]
            if i == 0:
                zts, ots = [zt], [ot]
            elif i == 1:
                zts.append(zt); ots.append(ot)
            i += 1
            k.dma("sp", zt[:rr, :], zp[r0:r0 + rr, c0:c0 + CH + 2])
            k.ts("dve", ot[:rr, :], zt[:rr, 0:CH], w[:, 0:1], ALU.mult, w[:, 3:4], ALU.add)
            k.stt("dve", ot[:rr, :], zt[:rr, 1:CH + 1], w[:, 1:2], ot[:rr, :], ALU.mult, ALU.add)
            k.stt("dve", ot[:rr, :], zt[:rr, 2:CH + 2], w[:, 2:3], ot[:rr, :], ALU.mult, ALU.add)
            k.dma("sp", zo[r0:r0 + rr, c0:c0 + CH], ot[:rr, :])
        r0 += rr
    return k.finish()


def build_HF(n):
    k = KB()
    TW = min(512, n)
    NT = n // TW
    zT = k.dram("zT", [2, 33, n], F32, "ExternalInput")
    w1 = k.dram("w1", [33, 64], F32, "ExternalInput")
    w2 = k.dram("w2", [64, 64], F32, "ExternalInput")
    w3 = k.dram("w3", [2, 64, 2 * CPC], F32, "ExternalInput")
    pv = k.dram("pv", [64, 4], F32, "ExternalInput")
    dec = k.dram("dec", [2, 2 * CPC, n], F32, "ExternalInput")
    G = k.dram("G", [2, CPC, 2 * n], BF16, "ExternalOutput")
    w1s = k.sb([33, 64], F32); k.dma("sp", w1s[:], w1[:, :])
    w2s = k.sb([64, 64], F32); k.dma("sp", w2s[:], w2[:, :])
    w3s = k.sb([64, 2, 2 * CPC], F32); k.dma("sp", w3s[:], w3.v(w3.t.rearrange("d k m -> k d m")))
    pvs = k.sb([64, 4], F32); k.dma("sp", pvs[:], pv[:, :])
    H = [k.sb([2 * CPC, n], F32, "H%d" % d) for d in range(2)]
    acc = k.sb([2 * CPC, 2, NT], F32); k.memset("dve", acc[:], 0.0)
    p1 = k.ps([64, TW], F32); p2 = k.ps([64, TW], F32); p3 = k.ps([2 * CPC, TW], F32)
    zt = [k.sb([33, TW], F32, "zt%d" % i) for i in range(2)]
    dt_ = [k.sb([2 * CPC, TW], F32, "dt%d" % i) for i in range(2)]
    x = k.sb([64, TW], F32); yi = k.sb([64, TW], I32); yf = k.sb([64, TW], F32); gg = k.sb([64, TW], F32)
    hid = k.sb([64, TW], F32); hid2 = k.sb([64, TW], F32); junk = k.sb([2 * CPC, TW], F32)

    def sin_layer(out, ps, bcol, fcol):
        k.ts("dve", x[:], ps[:], pvs[:, bcol:bcol + 1], ALU.add, pvs[:, fcol:fcol + 1], ALU.mult)
        k.ts("dve", x[:], x[:], 1.0 / (2 * math.pi), ALU.mult, 8.0, ALU.add)
        k.copy("dve", yi[:], x[:])
        k.copy("dve", yf[:], yi[:])
        k.tt("dve", x[:], x[:], yf[:], ALU.subtract)
        k.ts("dve", gg[:], x[:], 0.5, ALU.is_gt)
        k.tt("dve", x[:], x[:], gg[:], ALU.subtract)
        k.act(out[:], x[:], AF.Sin, scale=2 * math.pi)

    it = 0
    for d in range(2):
        for t in range(NT):
            sl = slice(t * TW, (t + 1) * TW)
            b = it % 2; it += 1
            k.dma("sp", zt[b][:], zT[d, :, sl])
            k.dma("sp", dt_[b][:], dec[d, :, sl])
            k.mm(p1[:], w1s[:], zt[b][:])
            sin_layer(hid, p1, 0, 1)
            k.mm(p2[:], w2s[:], hid[:])
            sin_layer(hid2, p2, 2, 3)
            k.mm(p3[:], w3s[:, d, :], hid2[:])
            k.copy("act", H[d][:, sl], p3[:])
            k.tt("dve", H[d][:, sl], H[d][:, sl], dt_[b][:], ALU.mult)
            k.act(junk[:], H[d][:, sl], AF.Abs, accum=acc[:, d, t:t + 1])
    tot = k.sb([2 * CPC, 2], F32)
    k.reduce("dve", tot[:], acc[:], ALU.add)
    k.recip(tot[:], tot[:])
    ob = [k.sb([2 * CPC, TW], BF16, "ob%d" % i) for i in range(2)]
    it = 0
    for d in range(2):
        for t in range(NT):
            sl = slice(t * TW, (t + 1) * TW)
            b = it % 2; it += 1
            k.ts("dve", ob[b][:], H[d][:, sl], tot[:, d:d + 1], ALU.mult)
            off = (n if d == 0 else 0) + t * TW
            for o in range(2):
                k.dma("sp", G[o, :, off:off + TW], ob[b][o * CPC:(o + 1) * CPC, :])
    return k.finish()


def build_HC(n, CG=16):
    k = KB()
    nb = n // 128
    CG = min(CG, CPC)
    G = k.dram("G", [CPC, 2 * n], BF16, "ExternalInput")
    Ur = k.dram("Ur", [128, CPC, 2, nb], F32, "ExternalInput")
    Vn = k.dram("Vn", [128, CPC, 2, nb], F32, "ExternalInput")
    X = k.dram("X", [128, CPC, 2, nb], F32, "ExternalInput")
    Dbc = k.dram("Dbc", [128, CPC], F32, "ExternalInput")
    Y = k.dram("Y", [128, CPC, 2, nb], F32, "ExternalOutput")
    dsb = k.sb([128, CPC], F32); k.dma("sp", dsb[:], Dbc[:, :])
    uf = k.sb([128, CG, 2, nb], F32); ub = k.sb([128, CG, 2, nb], BF16)
    vn = k.sb([128, CG, 2, nb], F32); xg = k.sb([128, CG, 2, nb], F32); yo = k.sb([128, CG, 2, nb], F32)
    L = 2 * n - 128
    TT = [k.sb([128, L], BF16, "TT%d" % i) for i in range(2)]
    Yp = [k.ps([128, 2, nb], F32, "Yp%d" % i) for i in range(2)]
    ysb = [k.sb([128, 2, nb], F32, "ysb%d" % i) for i in range(2)]
    deltas = [0] + [d for d in range(-(nb - 1), nb) if d != 0]
    ci = 0
    for g0 in range(0, CPC, CG):
        k.dma("pool", uf[:], Ur[:, g0:g0 + CG, :, :])
        k.copy("dve", ub[:], uf[:])
        k.dma("pool", vn[:], Vn[:, g0:g0 + CG, :, :])
        k.dma("pool", xg[:], X[:, g0:g0 + CG, :, :])
        for cc in range(CG):
            c = g0 + cc
            tt = TT[ci % 2]; yp = Yp[ci % 2]; ys = ysb[ci % 2]; ci += 1
            k.dma("sp", tt[:], G.v(bass.AP(tensor=G.t, offset=c * 2 * n + 1, ap=[[1, 128], [1, L]])))
            for i, dl in enumerate(deltas):
                jj0 = 128 * (dl + nb - 1)
                if dl >= 0:
                    blo, bhi, alo = 0, nb - dl, dl
                else:
                    blo, bhi, alo = -dl, nb, 0
                N = bhi - blo
                k.mm(yp[:, :, alo:alo + N], tt[:, jj0:jj0 + 128], ub[:, cc, :, blo:bhi],
                     start=(i == 0), stop=(i == len(deltas) - 1))
            k.copy("act", ys[:], yp[:])
            k.stt("dve", ys[:], vn[:, cc, :, :], dsb[:, c:c + 1], ys[:], ALU.mult, ALU.add)
            k.tt("dve", yo[:, cc, :, :], ys[:], xg[:, cc, :, :], ALU.mult)
        k.dma("sp", Y[:, g0:g0 + CG, :, :], yo[:])
    return k.finish()


def hyena_consts(n, ch0):
    t = np.linspace(0.0, 1.0, n, dtype=np.float32)[:, None]
    bands = 16
    w = (np.float32(2.0 * math.pi / n) * np.arange(n, dtype=np.float32))[:, None]
    f = np.linspace(1e-4, bands - 1, bands, dtype=np.float32)[None, :]
    z = np.concatenate([t, np.cos(f * w), -np.sin(f * w)], -1).astype(np.float32)
    deltas = np.abs(np.linspace(math.log(1e-2) / 1.5, math.log(1e-2) / 0.3, 256, dtype=np.float32))
    dec = np.exp(-t * deltas[None, :]).astype(np.float32)
    dsel = dec[:, ch0:ch0 + CPC].T
    d2 = np.concatenate([dsel, dsel], 0)
    zT = np.stack([z.T, z[::-1].T]).astype(np.float32)
    dd = np.stack([d2, d2[:, ::-1]]).astype(np.float32)
    return np.ascontiguousarray(zT), np.ascontiguousarray(dd)


def to_blocks(u, rev=False):
    C, B, n = u.shape
    a = u.reshape(C, B, n // 128, 128)
    if rev:
        a = a[..., ::-1]
    return np.ascontiguousarray(a.transpose(3, 0, 1, 2))


def from_blocks(y):
    q, C, B, nb = y.shape
    return np.ascontiguousarray(y.transpose(1, 2, 3, 0).reshape(C, B, nb * 128))


import math

D = 1024
KC = 8
EPS = 1e-6


def build_C(T, lam_init):
    k = KB()
    TW = min(512, T)
    NT = T // TW
    hT = k.dram("hT", [D, T], F32, "ExternalInput")
    mods = k.dram("mods", [128, 48], F32, "ExternalInput")
    gmix = k.dram("gmix", [128, KC], F32, "ExternalInput")
    gffn = k.dram("gffn", [128, KC], F32, "ExternalInput")
    odT = k.dram("odT", [8, 128, T], F32, "ExternalInput")
    dlam = k.dram("dlam", [1, 256], F32, "ExternalInput")
    gsub = k.dram("gsub", [128, 1], F32, "ExternalInput")
    yhT = k.dram("yhT", [256, T], F32, "ExternalInput")
    ymT = k.dram("ymT", [256, T], F32, "ExternalInput")
    glup = k.dram("glup", [256, T + 30], F32, "ExternalInput")
    cw = k.dram("cw", [128, 2 * 31], F32, "ExternalInput")
    lnp = k.dram("lnp", [128, 4], F32, "ExternalInput")
    wg = k.dram("wg", [D, 4096], F32, "ExternalInput")
    wbr = k.dram("wbr", [1280, D], F32, "ExternalInput")
    wo = k.dram("wo", [D, D], F32, "ExternalInput")
    wr = k.dram("wr", [D, 16], F32, "ExternalInput")
    hmidT = k.dram("hmidT", [D, T], F32, "ExternalOutput")
    u2T = k.dram("u2T", [D, T], BF16, "ExternalOutput")
    aff = k.dram("aff", [T, 16], F32, "ExternalOutput")

    ones = k.sb([128, 128], F32); k.memset("dve", ones[:], 1.0)
    modt = k.sb([128, 48], F32); k.dma("sp", modt[:], mods[:, :])
    gm = k.sb([128, KC], F32); k.dma("sp", gm[:], gmix[:, :])
    gf = k.sb([128, KC], F32); k.dma("sp", gf[:], gffn[:, :])
    A1 = k.sb([128, KC], F32); A2 = k.sb([128, KC], F32)
    k.ts("dve", A1[:], modt[:, 8:16], 1.0, ALU.add); k.tt("dve", A1[:], A1[:], gm[:], ALU.mult)
    k.ts("dve", A2[:], modt[:, 32:40], 1.0, ALU.add); k.tt("dve", A2[:], A2[:], gf[:], ALU.mult)
    dl = k.sb([128, 256], F32)
    k.dma("sp", dl[:], dlam.v(bass.AP(tensor=dlam.t, offset=0, ap=[[0, 128], [1, 256]])))
    pr_ = k.sb([128, 2, 64], F32); e12 = k.sb([128, 2], F32); nlam = k.sb([128, 1], F32)
    k.tt("dve", pr_[:, 0, :], dl[:, 0:64], dl[:, 64:128], ALU.mult)
    k.tt("dve", pr_[:, 1, :], dl[:, 128:192], dl[:, 192:256], ALU.mult)
    k.reduce("dve", e12[:], pr_[:], ALU.add)
    k.act(e12[:], e12[:], AF.Exp)
    k.tt("dve", nlam[:], e12[:, 1:2], e12[:, 0:1], ALU.subtract)
    k.ts("dve", nlam[:], nlam[:], -lam_init, ALU.add)
    gs = k.sb([128, 1], F32); k.dma("sp", gs[:], gsub[:, :])
    k.ts("dve", gs[:], gs[:], 1.0 - lam_init, ALU.mult)
    cws = k.sb([128, 2, 31], F32); k.dma("sp", cws[:], cw.v(cw.t.rearrange("p (j t) -> p j t", j=2)))
    lns = k.sb([128, 4], F32); k.dma("sp", lns[:], lnp[:, :])
    wrs = k.sb([128, KC, 16], F32); k.dma("sp", wrs[:], wr.v(wr.t.rearrange("(c p) e -> p c e", p=128)))

    wgb = k.sb([128, KC, 4096], BF16, "wgb"); wbb = k.sb([128, 10, D], BF16, "wbb"); wob = k.sb([128, KC, D], BF16, "wob")
    stg = [k.sb([128, KC, 128], F32, "stg%d" % i) for i in range(2)]
    n = 0
    wg_v = wg.t.rearrange("(c p) f -> p c f", p=128); wo_v = wo.t.rearrange("(c p) f -> p c f", p=128)
    wb_v = wbr.t.rearrange("(c p) f -> p c f", p=128)
    for c0 in range(0, 4096, 128):
        s_ = stg[n % 2]; n += 1
        k.dma("pool", s_[:], wg.v(wg_v[:, :, c0:c0 + 128])); k.copy("dve", wgb[:, :, c0:c0 + 128], s_[:])
    for c0 in range(0, D, 128):
        s_ = stg[n % 2]; n += 1
        k.dma("pool", s_[:], wo.v(wo_v[:, :, c0:c0 + 128])); k.copy("dve", wob[:, :, c0:c0 + 128], s_[:])
    for c0 in range(0, D, 128):
        for r0 in (0, 5):
            s_ = stg[n % 2]; n += 1
            k.dma("pool", s_[:, 0:5, :], wbr.v(wb_v[:, r0:r0 + 5, c0:c0 + 128])); k.copy("dve", wbb[:, r0:r0 + 5, c0:c0 + 128], s_[:, 0:5, :])

    B1 = k.sb([128, KC, TW], F32, "B1"); B2 = k.sb([128, KC, TW], F32, "B2")
    ub = k.sb([128, KC, TW], BF16, "ub"); accb = k.sb([128, KC, TW], BF16, "accb")
    rstd = k.sb([128, TW], F32, "rstd"); tmp = k.sb([128, TW], F32, "tmp")
    o0 = k.sb([128, TW], F32); o1 = k.sb([128, TW], F32)
    ydb = k.sb([128, 4, TW], BF16); yhf = k.sb([128, 2, TW], F32); yhb = k.sb([128, 2, TW], BF16)
    ymf = yhf; ymb = k.sb([128, 2, TW], BF16); ycb = k.sb([128, 2, TW], BF16)
    glt = k.sb([128, TW + 30], F32); ucv = k.sb([128, 2, TW], F32)
    mean = k.sb([128, TW], F32); msq = k.sb([128, TW], F32)
    sg = k.sb([128, TW], F32); pb = k.sb([128, TW], F32); accf = k.sb([128, TW], F32)
    lg = k.sb([128, 16], F32); mx = k.sb([128, 1], F32); sm = k.sb([128, 1], F32); ex = k.sb([128, 16], F32)
    ps1 = k.ps([128, TW], F32); ps2 = k.ps([128, TW], F32)
    pg = [k.ps([128, TW], F32, "pg%d" % i) for i in range(2)]
    pp = [k.ps([128, TW], F32, "pp%d" % i) for i in range(2)]
    pl = k.ps([128, 16], F32)
    hT_v = hT.t.rearrange("(c p) t -> p c t", p=128)
    hm_v = hmidT.t.rearrange("(c p) t -> p c t", p=128)
    u2_v = u2T.t.rearrange("(c p) t -> p c t", p=128)
    ybr = [(ydb, 4, 0), (yhb, 2, 4), (ycb, 2, 6), (ymb, 2, 8)]

    def rstd_from(ps, n_feat):
        k.ts("dve", rstd[:], ps[:], 1.0 / n_feat, ALU.mult, EPS, ALU.add)
        k.act(rstd[:], rstd[:], AF.Sqrt)
        k.recip(rstd[:], rstd[:])

    def norm_mod(src, dst_f, dst_b, A, shift_col):
        k.act(B2[:], src[:], AF.Square)
        for c in range(KC):
            k.mm(ps1[:], ones[:], B2[:, c, :], start=(c == 0), stop=(c == KC - 1))
        rstd_from(ps1, D)
        for c in range(KC):
            k.tt("dve", tmp[:], src[:, c, :], rstd[:], ALU.mult)
            if dst_f is not None:
                k.ts("dve", dst_f[:, c, :], tmp[:], A[:, c:c + 1], ALU.mult, modt[:, shift_col + c:shift_col + c + 1], ALU.add)
                k.copy("dve", dst_b[:, c, :], dst_f[:, c, :])
            else:
                k.ts("dve", dst_b[:, c, :], tmp[:], A[:, c:c + 1], ALU.mult, modt[:, shift_col + c:shift_col + c + 1], ALU.add)

    for ti in range(NT):
        ts_ = slice(ti * TW, (ti + 1) * TW)
        k.dma("sp", B1[:], hT.v(hT_v[:, :, ts_]))
        norm_mod(B1, None, ub, A1, 0)
        for h in range(4):
            k.dma("sp", o0[:], odT[2 * h, :, ts_]); k.dma("sp", o1[:], odT[2 * h + 1, :, ts_])
            k.stt("dve", o0[:], o1[:], nlam[:, 0:1], o0[:], ALU.mult, ALU.add)
            k.act(o1[:], o0[:], AF.Square)
            k.mm(ps1[:], ones[:], o1[:])
            rstd_from(ps1, 128)
            k.stt("dve", ydb[:, h, :], o0[:], gs[:, 0:1], rstd[:], ALU.mult, ALU.mult)
        for j in range(2):
            k.dma("sp", glt[:], glup[j * 128:(j + 1) * 128, ti * TW: ti * TW + TW + 30])
            k.ts("dve", ucv[:, j, :], glt[:, 0:TW], cws[:, j, 0:1], ALU.mult)
            for t in range(1, 31):
                k.stt("dve", ucv[:, j, :], glt[:, t:t + TW], cws[:, j, t:t + 1], ucv[:, j, :], ALU.mult, ALU.add)
        k.act(B2[:, 0:2, :], ucv[:], AF.Square)
        for j in range(2):
            k.mm(ps1[:], ones[:], ucv[:, j, :], start=(j == 0), stop=(j == 1))
        for j in range(2):
            k.mm(ps2[:], ones[:], B2[:, j, :], start=(j == 0), stop=(j == 1))
        k.ts("dve", mean[:], ps1[:], 1.0 / 256, ALU.mult)
        k.tt("dve", msq[:], mean[:], mean[:], ALU.mult)
        k.ts("dve", rstd[:], ps2[:], 1.0 / 256, ALU.mult, EPS, ALU.add)
        k.tt("dve", rstd[:], rstd[:], msq[:], ALU.subtract)
        k.act(rstd[:], rstd[:], AF.Sqrt)
        k.recip(rstd[:], rstd[:])
        for j in range(2):
            k.tt("dve", tmp[:], ucv[:, j, :], mean[:], ALU.subtract)
            k.tt("dve", tmp[:], tmp[:], rstd[:], ALU.mult)
            k.ts("dve", tmp[:], tmp[:], lns[:, j:j + 1], ALU.mult, lns[:, 2 + j:3 + j], ALU.add)
            k.act(ycb[:, j, :], tmp[:], AF.Silu)
        k.dma("sp", yhf[:], yhT.v(yhT.t.rearrange("(j p) t -> p j t", p=128)[:, :, ts_])); k.copy("dve", yhb[:], yhf[:])
        k.dma("sp", ymf[:], ymT.v(ymT.t.rearrange("(j p) t -> p j t", p=128)[:, :, ts_])); k.copy("dve", ymb[:], ymf[:])
        it = 0
        for fc in range(KC):
            for i, (yt, nch, r0) in enumerate(ybr):
                g_ = pg[it % 2]; p_ = pp[it % 2]; it += 1
                for c in range(KC):
                    k.mm(g_[:], wgb[:, c, i * 1024 + fc * 128: i * 1024 + (fc + 1) * 128], ub[:, c, :], start=(c == 0), stop=(c == KC - 1))
                for c in range(nch):
                    k.mm(p_[:], wbb[:, r0 + c, fc * 128:(fc + 1) * 128], yt[:, c, :], start=(c == 0), stop=(c == nch - 1))
                k.act(sg[:], g_[:], AF.Sigmoid)
                k.copy("act", pb[:], p_[:])
                if i == 0:
                    k.tt("dve", accf[:], sg[:], pb[:], ALU.mult)
                else:
                    k.tt("dve", sg[:], sg[:], pb[:], ALU.mult)
                    k.tt("dve", accf[:], accf[:], sg[:], ALU.add)
            k.copy("dve", accb[:, fc, :], accf[:])
        for fo in range(KC):
            g_ = pg[fo % 2]
            for c in range(KC):
                k.mm(g_[:], wob[:, c, fo * 128:(fo + 1) * 128], accb[:, c, :], start=(c == 0), stop=(c == KC - 1))
            k.ts("dve", tmp[:], g_[:], modt[:, 16 + fo:17 + fo], ALU.mult)
            k.tt("dve", B1[:, fo, :], B1[:, fo, :], tmp[:], ALU.add)
        k.dma("sp", hmidT.v(hm_v[:, :, ts_]), B1[:])
        norm_mod(B1, B2, ub, A2, 24)
        k.dma("sp", u2T.v(u2_v[:, :, ts_]), ub[:])
        for s in range(TW // 128):
            for c in range(KC):
                k.mm(pl[:], B2[:, c, s * 128:(s + 1) * 128], wrs[:, c, :], start=(c == 0), stop=(c == KC - 1))
            k.copy("act", lg[:], pl[:])
            k.reduce("dve", mx[:], lg[:], ALU.max)
            k.ts("dve", mx[:], mx[:], -1.0, ALU.mult)
            k.act(ex[:], lg[:], AF.Exp, bias=mx[:, 0:1], accum=sm[:])
            k.recip(sm[:], sm[:])
            k.ts("dve", ex[:], ex[:], sm[:, 0:1], ALU.mult)
            k.dma("sp", aff[ti * TW + s * 128: ti * TW + (s + 1) * 128, :], ex[:])
    return k.finish()


def lay(v):
    v = np.asarray(v, np.float32).reshape(-1, 128)
    return np.ascontiguousarray(v.T)


D = 1024
KC = 8
EPS = 1e-6
NE = 16


def build_D(T, nseq, cap, final, ST=1536, nexp=NE):
    k = KB()
    TW = min(512, T)
    hmidT = k.dram("hmidT", [D, T], F32, "ExternalInput")
    u2T = k.dram("u2T", [D, T], BF16, "ExternalInput")
    affall = k.dram("affall", [128, nseq // 8], F32, "ExternalInput")
    affT = k.dram("affT", [NE, T], F32, "ExternalInput")
    mods = k.dram("mods", [128, 48], F32, "ExternalInput")
    win = k.dram("win", [NE, D, 2048], F32, "ExternalInput")
    wout = k.dram("wout", [NE, D, D], F32, "ExternalInput")
    mblk = k.dram("mblk", [128, 128], F32, "ExternalInput")
    sel = k.dram("sel", [128, NE], F32, "ExternalInput")
    selb = k.dram("selb", [NE, NE * 128], F32, "ExternalInput")
    gfin = k.dram("gfin", [128, KC], F32, "ExternalInput")
    houtT = k.dram("houtT", [D, T], F32, "ExternalOutput")

    modt = k.sb([128, 48], F32); k.dma("sp", modt[:], mods[:, :])
    mb = k.sb([128, 128], F32); k.dma("sp", mb[:], mblk[:, :])
    sl = k.sb([128, NE], F32); k.dma("sp", sl[:], sel[:, :])
    slb = k.sb([NE, NE, 128], F32); k.dma("sp", slb[:], selb.v(selb.t.rearrange("a (e m) -> a e m", e=NE)))
    gfs = k.sb([128, KC], F32); k.dma("sp", gfs[:], gfin[:, :])
    ones = k.sb([128, 128], F32); k.memset("dve", ones[:], 1.0)
    W = nseq // 8
    hb = k.sb([128, KC, TW], F32, "hb")
    hflat = hb.t[:].rearrange("p a b -> p (a b)")
    aa_v = hb.v(hflat[:, 0:W]); cmpb_v = hb.v(hflat[:, W:2 * W])
    k.dma("sp", aa_v, affall[:, :])
    lo = k.sb([128, 1], F32); hi = k.sb([128, 1], F32); mid = k.sb([128, 1], F32)
    cnt = k.sb([128, 1], F32); cc = k.sb([128, 1], F32); d1 = k.sb([128, 1], F32)
    k.memset("dve", lo[:], 0.0); k.memset("dve", hi[:], 1.0)
    pm = k.ps([128, TW], F32, "pm")
    for it in range(30):
        k.tt("dve", mid[:], lo[:], hi[:], ALU.add)
        k.ts("dve", mid[:], mid[:], 0.5, ALU.mult)
        k.ts("dve", cmpb_v, aa_v, mid[:, 0:1], ALU.is_ge)
        k.reduce("dve", cnt[:], cmpb_v, ALU.add)
        k.mm(pm[:, 0:1], mb[:], cnt[:])
        k.ts("dve", cc[:], pm[:, 0:1], float(cap) - 0.5, ALU.is_ge)
        k.tt("dve", d1[:], hi[:], mid[:], ALU.subtract)
        k.stt("dve", hi[:], d1[:], cc[:, 0:1], mid[:], ALU.mult, ALU.add)
        k.tt("dve", d1[:], mid[:], lo[:], ALU.subtract)
        k.stt("dve", lo[:], d1[:], cc[:, 0:1], lo[:], ALU.mult, ALU.add)
    k.mm(pm[:NE, 0:1], sl[:], lo[:])
    thr = k.sb([NE, 1], F32); k.copy("act", thr[:], pm[:NE, 0:1])
    mg = k.sb([NE, T], F32); k.dma("sp", mg[:], affT[:, :])
    mtmp = k.sb([NE, TW], F32)
    for t0 in range(0, T, TW):
        k.ts("dve", mtmp[:], mg[:, t0:t0 + TW], thr[:, 0:1], ALU.is_ge)
        k.tt("dve", mg[:, t0:t0 + TW], mg[:, t0:t0 + TW], mtmp[:], ALU.mult)

    ST = min(ST, T)
    acc = k.sb([128, KC, ST], F32, "acc"); u2 = k.sb([128, KC, ST], BF16, "u2")
    wib = k.sb([128, KC, 2048], BF16, "wib"); wob = k.sb([128, KC, D], BF16, "wob")
    stg = [k.sb([128, KC, 128], F32, "stg%d" % i) for i in range(2)]
    actb = k.sb([128, KC, TW], BF16, "actb")
    sgt = [k.sb([128, TW], F32, "sgt%d" % i) for i in range(2)]
    put = [k.sb([128, TW], F32, "put%d" % i) for i in range(2)]
    ysb = [k.sb([128, TW], F32, "ysb%d" % i) for i in range(2)]
    mgbs = k.sb([128, TW], F32, "mgbs")
    pgt = [k.ps([128, TW], F32, "pgt%d" % i) for i in range(2)]
    ppu = [k.ps([128, TW], F32, "ppu%d" % i) for i in range(2)]
    py = [k.ps([128, TW], F32, "py%d" % i) for i in range(2)]
    u2_v = u2T.t.rearrange("(c p) t -> p c t", p=128)
    hm_v = hmidT.t.rearrange("(c p) t -> p c t", p=128)
    ho_v = houtT.t.rearrange("(c p) t -> p c t", p=128)
    rstd = k.sb([128, TW], F32)
    n = 0
    for s0 in range(0, T, ST):
        sw = min(ST, T - s0)
        k.dma("sp", u2[:, :, 0:sw], u2T.v(u2_v[:, :, s0:s0 + sw]))
        for e in range(nexp):
            wi_v = win.t[e].rearrange("(c p) f -> p c f", p=128); wo_v = wout.t[e].rearrange("(c p) f -> p c f", p=128)
            for c0 in range(0, 2048, 128):
                s_ = stg[n % 2]; n += 1
                k.dma("pool", s_[:], win.v(wi_v[:, :, c0:c0 + 128])); k.copy("dve", wib[:, :, c0:c0 + 128], s_[:])
            for c0 in range(0, D, 128):
                s_ = stg[n % 2]; n += 1
                k.dma("pool", s_[:], wout.v(wo_v[:, :, c0:c0 + 128])); k.copy("dve", wob[:, :, c0:c0 + 128], s_[:])
            for t0 in range(0, sw, TW):
                tl = slice(t0, t0 + TW)
                k.mm(pm[:], slb[:, e, :], mg[:, s0 + t0: s0 + t0 + TW])
                k.copy("act", mgbs[:], pm[:])
                for hc in range(KC):
                    b = hc % 2
                    for c in range(KC):
                        k.mm(pgt[b][:], wib[:, c, hc * 128:(hc + 1) * 128], u2[:, c, tl], start=(c == 0), stop=(c == KC - 1))
                    for c in range(KC):
                        k.mm(ppu[b][:], wib[:, c, 1024 + hc * 128:1024 + (hc + 1) * 128], u2[:, c, tl], start=(c == 0), stop=(c == KC - 1))
                    k.act(sgt[b][:], pgt[b][:], AF.Silu)
                    k.copy("act", put[b][:], ppu[b][:])
                    k.tt("dve", actb[:, hc, :], sgt[b][:], put[b][:], ALU.mult)
                for dc in range(KC):
                    b = dc % 2
                    for c in range(KC):
                        k.mm(py[b][:], wob[:, c, dc * 128:(dc + 1) * 128], actb[:, c, :], start=(c == 0), stop=(c == KC - 1))
                    k.copy("act", ysb[b][:], py[b][:])
                    if e == 0:
                        k.tt("dve", acc[:, dc, tl], ysb[b][:], mgbs[:], ALU.mult)
                    else:
                        k.tt("dve", ysb[b][:], ysb[b][:], mgbs[:], ALU.mult)
                        k.tt("dve", acc[:, dc, tl], acc[:, dc, tl], ysb[b][:], ALU.add)
        for t0 in range(0, sw, TW):
            tl = slice(t0, t0 + TW); gl = slice(s0 + t0, s0 + t0 + TW)
            k.dma("sp", hb[:], hmidT.v(hm_v[:, :, gl]))
            for dc in range(KC):
                k.stt("dve", hb[:, dc, :], acc[:, dc, tl], modt[:, 40 + dc:41 + dc], hb[:, dc, :], ALU.mult, ALU.add)
            if final:
                sqb = acc
                k.act(sqb[:, :, tl], hb[:], AF.Square)
                for c in range(KC):
                    k.mm(pm[:], ones[:], sqb[:, c, tl], start=(c == 0), stop=(c == KC - 1))
                k.ts("dve", rstd[:], pm[:], 1.0 / D, ALU.mult, EPS, ALU.add)
                k.act(rstd[:], rstd[:], AF.Sqrt)
                k.recip(rstd[:], rstd[:])
                for c in range(KC):
                    k.stt("dve", hb[:, c, :], hb[:, c, :], gfs[:, c:c + 1], rstd[:], ALU.mult, ALU.mult)
            k.dma("sp", houtT.v(ho_v[:, :, gl]), hb[:])
    return k.finish()


def D_consts():
    mblk = np.zeros((128, 128), np.float32)
    for p in range(128):
        mblk[p, (p // 8) * 8:(p // 8) * 8 + 8] = 1.0
    sel = np.zeros((128, NE), np.float32)
    for e in range(NE):
        sel[8 * e, e] = 1.0
    selb = np.zeros((NE, NE, 128), np.float32)
    for e in range(NE):
        selb[e, e, :] = 1.0
    return dict(mblk=mblk, sel=sel, selb=selb.reshape(NE, NE * 128))


def aff_layout(aff):
    nseq = aff.shape[0]
    return np.ascontiguousarray(aff.T.reshape(NE, 8, nseq // 8).reshape(128, nseq // 8))


def lay(v):
    v = np.asarray(v, np.float32).reshape(-1, 128)
    return np.ascontiguousarray(v.T)


SEQ = 16384
BATCH = 2
CTX = 256
NCORE = 8
TC = SEQ // 4
_PROG = {}


def _prog(key, fn):
    if key not in _PROG:
        _PROG[key] = fn()
    return _PROG[key]


def _run(nc, maps):
    res = run_bass_kernel_spmd(nc, maps, core_ids=list(range(NCORE)))
    return [{kk: np.asarray(v) for kk, v in r.items()} for r in res.results]


def _c(a, dt=np.float32):
    return np.ascontiguousarray(a, dtype=dt)


def _hyena(l, inputs, zsrc, n):
    p = {kk: np.asarray(inputs[kk][l], np.float32) for kk in
         ("hyena_short_w", "hyena_short_b", "filt_w1", "filt_b1", "filt_freq", "filt_w2", "filt_b2", "filt_w3", "hyena_skip")}
    mapsS, mapsF = [], []
    for i in range(NCORE):
        ch0 = CPC * i
        rows = np.concatenate([np.arange(ch0, ch0 + CPC) + s * 256 for s in range(3)])
        zr = np.stack([zsrc[b][rows] for b in range(BATCH)], 1)
        zpad = np.pad(zr.reshape(96 * 2, n), ((0, 0), (1, 1)))
        wsb = np.repeat(np.concatenate([p["hyena_short_w"][:, rows].T, p["hyena_short_b"][rows][:, None]], 1), 2, axis=0)
        mapsS.append({"zp": _c(zpad), "wsb": _c(wsb)})
        zT, dd = hyena_consts(n, ch0)
        cols = lambda d: np.concatenate([o * 512 + d * 256 + np.arange(ch0, ch0 + CPC) for o in range(2)])
        w3 = np.stack([p["filt_w3"][:, cols(0)], p["filt_w3"][:, cols(1)]])
        pv = np.stack([p["filt_b1"], p["filt_freq"][0], p["filt_b2"], p["filt_freq"][1]], 1)
        mapsF.append({"zT": zT, "w1": _c(p["filt_w1"]), "w2": _c(p["filt_w2"]), "w3": _c(w3), "pv": _c(pv), "dec": dd})
    rS = _run(_prog(("HS", n), lambda: build_HS(192, n)), mapsS)
    rF = _run(_prog(("HF", n), lambda: build_HF(n)), mapsF)
    zs = [r["zo"].reshape(3, CPC, 2, n) for r in rS]
    cur = [z[2] for z in zs]
    for o in range(2):
        mapsC = []
        for i in range(NCORE):
            dsk = p["hyena_skip"][o, CPC * i:CPC * (i + 1)]
            mapsC.append({"G": np.ascontiguousarray(rF[i]["G"][o]), "Ur": to_blocks(cur[i], rev=True), "Vn": to_blocks(cur[i]),
                          "X": to_blocks(zs[i][o]), "Dbc": _c(np.broadcast_to(dsk[None, :], (128, CPC)))})
        rC = _run(_prog(("HC", n), lambda: build_HC(n)), mapsC)
        cur = [from_blocks(r["Y"]) for r in rC]
    return [np.concatenate([cur[i][:, b, :] for i in range(NCORE)], 0) for b in range(BATCH)]


def _attention(qd, kd, vd, mq, mk, mv, Tq, Tk):
    bf = qd[0].dtype
    maps = []
    for i in range(NCORE):
        b, h = divmod(i, 4)
        maps.append({"QT": np.ascontiguousarray(qd[b][h].reshape(2, 64, Tq)), "KT": np.ascontiguousarray(kd[b][h].reshape(2, 64, Tk)),
                     "V": np.ascontiguousarray(np.stack([vd[b][:, h * 128:(h + 1) * 128]] * 2))})
    r = _run(_prog(("Bd", Tq, Tk), lambda: build_attn(2, 64, 128, Tq, Tk, 0.125)), maps)
    od = [np.concatenate([r[b * 4 + h]["OT"] for h in range(4)], 0) for b in range(BATCH)]
    maps = []
    for i in range(NCORE):
        b, h = divmod(i, 4)
        maps.append({"QT": np.ascontiguousarray(mq[b][h][None]), "KT": np.ascontiguousarray(mk[b][h][None]),
                     "V": np.ascontiguousarray(mv[b][:, h * 64:(h + 1) * 64][None])})
    r = _run(_prog(("Bm", Tq, Tk), lambda: build_attn(1, 96, 64, Tq, Tk, 96 ** -0.5)), maps)
    om = [np.concatenate([r[b * 4 + h]["OT"][0] for h in range(4)], 0) for b in range(BATCH)]
    return od, om


def kernel(**inputs):
    inp = {kk: np.asarray(v) for kk, v in inputs.items()}
    x = inp["x"].astype(np.float32)
    cv = np.stack([lay(inp["c"][0]), lay(inp["c"][1]), lay(inp["c_ctx"])], -1).reshape(128, KC * 3)
    r0 = _run(_prog("P0", build_P0), [{"cvec": _c(cv), "ada_w": _c(inp["ada_w"]), "ada_b": np.stack([lay(inp["ada_b"][l]) for l in range(L)])}] * NCORE)
    mods = r0[0]["mods"].reshape(L, 128, 48, 3)
    h_lat = [x[b] for b in range(BATCH)]
    h_ctx = [inp["ctx"][b].astype(np.float32) for b in range(BATCH)]
    dconst = D_consts()
    out = np.zeros((BATCH, SEQ, 1024), np.float32)
    for l in range(L):
        last = l == L - 1
        lam_init = 0.8 - 0.6 * math.exp(-0.3 * l)
        w_in = inp["w_in"][l].astype(np.float32)
        aw = A_weights(w_in, inp["mla_w_uq"][l], inp["mla_w_ukv"][l], inp["mla_q_norm_g"][l], inp["mla_kv_norm_g"][l], inp["norm_mix_g"][l])
        mapsL, mapsX = [], []
        rc_ctx = rope_consts(np.arange(CTX), use_rope=False)
        for i in range(NCORE):
            b, j = divmod(i, 4)
            m = {"hT": _c(h_lat[b][j * TC:(j + 1) * TC].T), "mods": _c(mods[l, :, :, b])}
            m.update(rope_consts(np.arange(j * TC, (j + 1) * TC))); m.update(aw)
            mapsL.append(m)
            m = {"hT": _c(h_ctx[b].T), "mods": _c(mods[l, :, :, 2])}
            m.update(rc_ctx); m.update(aw)
            mapsX.append(m)
        rA = _run(_prog(("A", TC), lambda: build_A(TC)), mapsL)
        rX = _run(_prog(("A", CTX), lambda: build_A(CTX)), mapsX)
        cat = lambda name, b, ax: np.concatenate([rA[b * 4 + j][name] for j in range(4)], ax)
        qk_l = [cat("qkT", b, 2) for b in range(BATCH)]
        qk_c = [rX[b * 4]["qkT"] for b in range(BATCH)]
        v_all = [np.concatenate([rX[b * 4]["vtok"], cat("vtok", b, 0)], 0) for b in range(BATCH)]
        mq_l = [cat("mqT", b, 2) for b in range(BATCH)]
        mk_all = [np.concatenate([rX[b * 4]["mkT"], cat("mkT", b, 2)], 2) for b in range(BATCH)]
        mv_all = [np.concatenate([rX[b * 4]["mvtok"], cat("mvtok", b, 0)], 0) for b in range(BATCH)]
        kd_all = [np.concatenate([qk_c[b][4:8], qk_l[b][4:8]], 2) for b in range(BATCH)]
        od_l, om_l = _attention([qk_l[b][0:4] for b in range(BATCH)], kd_all, v_all, mq_l, mk_all, mv_all, SEQ, SEQ + CTX)
        yh_l = _hyena(l, inp, [cat("hyT", b, 1) for b in range(BATCH)], SEQ)
        glu_l = [np.pad(cat("gluT", b, 1), ((0, 0), (15, 15))) for b in range(BATCH)]
        if not last:
            od_c, om_c = _attention([qk_c[b][0:4] for b in range(BATCH)], [qk_c[b][4:8] for b in range(BATCH)],
                                    [rX[b * 4]["vtok"] for b in range(BATCH)], [rX[b * 4]["mqT"] for b in range(BATCH)],
                                    [rX[b * 4]["mkT"] for b in range(BATCH)], [rX[b * 4]["mvtok"] for b in range(BATCH)], CTX, CTX)
            yh_c = _hyena(l, inp, [rX[b * 4]["hyT"] for b in range(BATCH)], CTX)
            glu_c = [np.pad(rX[b * 4]["gluT"], ((0, 0), (15, 15))) for b in range(BATCH)]
        cwl = inp["conf_dw_w"][l].astype(np.float32)
        lng, lnb = inp["conf_ln_g"][l], inp["conf_ln_b"][l]
        cshared = {"gmix": lay(inp["norm_mix_g"][l]), "gffn": lay(inp["norm_ffn_g"][l]), "dlam": _c(inp["diff_lambda"][l].reshape(1, 256)),
                   "gsub": _c(inp["diff_subln_g"][l].reshape(128, 1)),
                   "cw": _c(cwl.T.reshape(2, 128, 31).transpose(1, 0, 2).reshape(128, 62)),
                   "lnp": _c(np.stack([lng[:128], lng[128:], lnb[:128], lnb[128:]], 1)),
                   "wg": _c(w_in[:, 3232:7328]), "wbr": _c(inp["w_branch"][l]), "wo": _c(inp["w_out"][l]), "wr": _c(inp["w_router"][l])}
        maps = []
        for i in range(NCORE):
            b, j = divmod(i, 4)
            tk = slice(j * TC, (j + 1) * TC)
            m = {"hT": _c(h_lat[b][tk].T), "mods": _c(mods[l, :, :, b]), "odT": _c(od_l[b][:, :, tk]), "yhT": _c(yh_l[b][:, tk]),
                 "ymT": _c(om_l[b][:, tk]), "glup": _c(glu_l[b][:, j * TC: (j + 1) * TC + 30])}
            m.update(cshared); maps.append(m)
        rC = _run(_prog(("C", TC, l), lambda: build_C(TC, lam_init)), maps)
        dshared = {"win": _c(inp["w_exp_in"][l]), "wout": _c(inp["w_exp_out"][l]), "gfin": lay(inp["final_norm_g"])}
        dshared.update(dconst)
        aff_b = [np.concatenate([rC[b * 4 + j]["aff"] for j in range(4)], 0) for b in range(BATCH)]
        maps = []
        for i in range(NCORE):
            b, j = divmod(i, 4)
            m = {"hmidT": rC[i]["hmidT"], "u2T": rC[i]["u2T"], "affall": aff_layout(aff_b[b]), "affT": _c(aff_b[b][j * TC:(j + 1) * TC].T),
                 "mods": _c(mods[l, :, :, b])}
            m.update(dshared); maps.append(m)
        rD = _run(_prog(("D", TC, last), lambda: build_D(TC, SEQ, 2 * SEQ // NE, last, ST=1536)), maps)
        for i in range(NCORE):
            b, j = divmod(i, 4)
            res = rD[i]["houtT"].T
            if last:
                out[b, j * TC:(j + 1) * TC] = res
            else:
                h_lat[b] = h_lat[b].copy() if j == 0 else h_lat[b]
                h_lat[b][j * TC:(j + 1) * TC] = res
        if not last:
            maps = []
            for i in range(NCORE):
                b = i // 4
                m = {"hT": _c(h_ctx[b].T), "mods": _c(mods[l, :, :, 2]), "odT": _c(od_c[b]), "yhT": _c(yh_c[b]), "ymT": _c(om_c[b]), "glup": _c(glu_c[b])}
                m.update(cshared); maps.append(m)
            rCc = _run(_prog(("C", CTX, l), lambda: build_C(CTX, lam_init)), maps)
            maps = []
            for i in range(NCORE):
                b = i // 4
                a_ = rCc[b * 4]["aff"]
                m = {"hmidT": rCc[i]["hmidT"], "u2T": rCc[i]["u2T"], "affall": aff_layout(a_), "affT": _c(a_.T), "mods": _c(mods[l, :, :, 2])}
                m.update(dshared); maps.append(m)
            rDc = _run(_prog(("D", CTX, False), lambda: build_D(CTX, CTX, 2 * CTX // NE, False, ST=1024)), maps)
            h_ctx = [rDc[b * 4]["houtT"].T.copy() for b in range(BATCH)]
    return out
```
